# Optimizing a Trainium2 kernel written in Bass

```python
import math
import jax
import jax.numpy as jnp
from jax import lax
import numpy as np


D_MODEL = 2048
BATCH = 4
SEQ = 2048
DEPTH = 4

N_MIXERS = 4
LAYERS_PER_MIXER = tuple(len(range(m, DEPTH, N_MIXERS)) for m in range(N_MIXERS))

MLA_HEADS = 16
MLA_Q_LORA = 512
MLA_KV_LORA = 512
MLA_NOPE = 128
MLA_ROPE = 64
MLA_V = 128
ROPE_THETA = 10000.0
ATTN_Q_BLOCK = 128

GDN_K_HEADS = 16
GDN_V_HEADS = 32
GDN_DK = 128
GDN_DV = 128
GDN_CONV = 4
GDN_CHUNK = 64

GLA_HEADS = 4
GLA_KEY_DIM = D_MODEL // 2
GLA_VAL_DIM = D_MODEL
GLA_GATE_RANK = 16
GLA_GATE_NORMALIZER = 16.0
GLA_CHUNK = 64

MOBA_HEADS = 16
MOBA_HEAD_DIM = D_MODEL // MOBA_HEADS
MOBA_BLOCK = 256
MOBA_TOPK = 3
MOBA_Q_BLOCK = 8

REL_BUCKETS = 32
REL_MAX_DIST = 128

N_EXPERTS = 32
TOP_K = 4
EXPERT_FF = 768
SWIGLU_LIMIT = 7.0
SWIGLU_ALPHA = 1.702

DEEPNORM_ALPHA = (2 * DEPTH) ** 0.25
DEEPNORM_BETA = (8 * DEPTH) ** -0.25
LN_EPS = 1e-5
RMS_EPS = 1e-6

kernel_name = "hybrid_mla_gdn_gla_moba_moe_trunk"


def layer_norm(x, g, b):
    xf = x.astype(jnp.float32)
    mu = jnp.mean(xf, -1, keepdims=True)
    var = jnp.mean(jnp.square(xf - mu), -1, keepdims=True)
    return ((xf - mu) * lax.rsqrt(var + LN_EPS)).astype(x.dtype) * g + b


def rms_norm(x, g):
    xf = x.astype(jnp.float32)
    y = xf * lax.rsqrt(jnp.mean(xf * xf, -1, keepdims=True) + RMS_EPS)
    return y.astype(x.dtype) * g


def l2_normalize(x):
    xf = x.astype(jnp.float32)
    return xf * lax.rsqrt(jnp.sum(xf * xf, -1, keepdims=True) + 1e-6)


def rope_tables(pos, dim):
    half = dim // 2
    inv_freq = ROPE_THETA ** (-jnp.arange(half, dtype=jnp.float32) / half)
    ang = pos.astype(jnp.float32)[:, None] * inv_freq[None, :]
    return jnp.cos(ang), jnp.sin(ang)


def apply_rope(x, cos, sin):
    half = x.shape[-1] // 2
    x1, x2 = x[..., :half], x[..., half:]
    return jnp.concatenate([x1 * cos - x2 * sin, x2 * cos + x1 * sin], -1).astype(x.dtype)


def t5_bucket(dist):
    n = jnp.maximum(dist, 0)
    max_exact = REL_BUCKETS // 2
    large = max_exact + (jnp.log(jnp.maximum(n, 1).astype(jnp.float32) / max_exact)
                         / math.log(REL_MAX_DIST / max_exact) * (REL_BUCKETS - max_exact)).astype(jnp.int32)
    large = jnp.minimum(large, REL_BUCKETS - 1)
    return jnp.where(n < max_exact, n, large)


def causal_depthwise_conv(x, w):
    k_width, ch = w.shape
    return lax.conv_general_dilated(x, w[:, None, :], window_strides=(1,), padding=[(k_width - 1, 0)],
                                    dimension_numbers=('NWC', 'WIO', 'NWC'), feature_group_count=ch)


def mla_mixer(u, pos, w_in, q_norm, kv_norm, w_qb, w_kvb, w_o):
    B, S, _ = u.shape
    lat = u @ w_in
    q_lat, kv_lat, k_rope = jnp.split(lat, [MLA_Q_LORA, MLA_Q_LORA + MLA_KV_LORA], -1)
    q = (rms_norm(q_lat, q_norm) @ w_qb).reshape(B, S, MLA_HEADS, MLA_NOPE + MLA_ROPE)
    kv = (rms_norm(kv_lat, kv_norm) @ w_kvb).reshape(B, S, MLA_HEADS, MLA_NOPE + MLA_V)
    q_nope, q_rope = q[..., :MLA_NOPE], q[..., MLA_NOPE:]
    k_nope, v = kv[..., :MLA_NOPE], kv[..., MLA_NOPE:]
    cos, sin = rope_tables(pos, MLA_ROPE)
    q_rope = apply_rope(q_rope, cos[:, None, :], sin[:, None, :])
    k_rope = apply_rope(k_rope, cos, sin)
    scale = (MLA_NOPE + MLA_ROPE) ** -0.5
    key_pos = jnp.arange(S)

    def block(i):
        s0 = i * ATTN_Q_BLOCK
        qn = lax.dynamic_slice_in_dim(q_nope, s0, ATTN_Q_BLOCK, 1)
        qr = lax.dynamic_slice_in_dim(q_rope, s0, ATTN_Q_BLOCK, 1)
        logits = (jnp.einsum('bqhd,bkhd->bhqk', qn, k_nope)
                  + jnp.einsum('bqhd,bkd->bhqk', qr, k_rope)).astype(jnp.float32) * scale
        q_pos = s0 + jnp.arange(ATTN_Q_BLOCK)
        logits = jnp.where(key_pos[None, :] <= q_pos[:, None], logits, -jnp.inf)
        p = jax.nn.softmax(logits, -1).astype(v.dtype)
        return jnp.einsum('bhqk,bkhd->bqhd', p, v)

    o = lax.map(block, jnp.arange(S // ATTN_Q_BLOCK))
    o = o.transpose(1, 0, 2, 3, 4).reshape(B, S, MLA_HEADS * MLA_V)
    return o @ w_o


def gated_delta_rule_chunked(q, k, v, g, beta):
    B, S, H, DK = q.shape
    DV = v.shape[-1]
    C = GDN_CHUNK
    N = S // C
    f32 = jnp.float32
    q = l2_normalize(q) * (DK ** -0.5)
    k = l2_normalize(k)
    v = v.astype(f32)

    def chunks(t):
        return t.reshape(B, N, C, H, *t.shape[3:]).swapaxes(2, 3)

    q, k, v, g, beta = chunks(q), chunks(k), chunks(v), chunks(g), chunks(beta)
    g = jnp.cumsum(g, -1)
    tri = jnp.tril(jnp.ones((C, C), bool))
    strict = jnp.tril(jnp.ones((C, C), bool), -1)
    decay = jnp.where(tri, jnp.exp(jnp.where(tri, g[..., :, None] - g[..., None, :], 0.0)), 0.0)
    k_beta = k * beta[..., None]
    v_beta = v * beta[..., None]
    a_low = jnp.where(strict, jnp.einsum('bnhid,bnhjd->bnhij', k_beta, k) * decay, 0.0)
    eye = jnp.eye(C, dtype=f32)
    rhs = jnp.concatenate([v_beta, k_beta * jnp.exp(g)[..., None]], -1)
    sol = lax.linalg.triangular_solve(a_low + eye, rhs, left_side=True, lower=True, unit_diagonal=True)
    u_val, w_dec = sol[..., :DV], sol[..., DV:]
    attn_intra = jnp.where(tri, jnp.einsum('bnhid,bnhjd->bnhij', q, k) * decay, 0.0)
    g_last = g[..., -1]
    q_dec = q * jnp.exp(g)[..., None]
    k_tail = k * jnp.exp(g_last[..., None] - g)[..., None]

    def step(state, xs):
        u_c, w_c, a_c, qd_c, kt_c, gl_c = xs
        v_new = u_c - jnp.einsum('bhcd,bhde->bhce', w_c, state)
        o_c = jnp.einsum('bhcd,bhde->bhce', qd_c, state) + jnp.einsum('bhij,bhje->bhie', a_c, v_new)
        state = state * jnp.exp(gl_c)[..., None, None] + jnp.einsum('bhcd,bhce->bhde', kt_c, v_new)
        return state, o_c

    xs = tuple(jnp.moveaxis(t, 1, 0) for t in (u_val, w_dec, attn_intra, q_dec, k_tail, g_last))
    _, o = lax.scan(step, jnp.zeros((B, H, DK, DV), f32), xs)
    return o.transpose(1, 0, 3, 2, 4).reshape(B, S, H, DV)


def gdn_mixer(u, w_in, conv_w, a_log, dt_bias, norm_g, w_o):
    B, S, _ = u.shape
    qk_dim = GDN_K_HEADS * GDN_DK
    v_dim = GDN_V_HEADS * GDN_DV
    proj = u @ w_in
    qkv, z, b, a = jnp.split(proj, [2 * qk_dim + v_dim, 2 * qk_dim + 2 * v_dim,
                                    2 * qk_dim + 2 * v_dim + GDN_V_HEADS], -1)
    qkv = jax.nn.silu(causal_depthwise_conv(qkv, conv_w))
    q, k, v = jnp.split(qkv, [qk_dim, 2 * qk_dim], -1)
    rep = GDN_V_HEADS // GDN_K_HEADS
    q = jnp.repeat(q.reshape(B, S, GDN_K_HEADS, GDN_DK), rep, axis=2)
    k = jnp.repeat(k.reshape(B, S, GDN_K_HEADS, GDN_DK), rep, axis=2)
    v = v.reshape(B, S, GDN_V_HEADS, GDN_DV)
    beta = jax.nn.sigmoid(b.astype(jnp.float32))
    g = -jnp.exp(a_log.astype(jnp.float32)) * jax.nn.softplus(a.astype(jnp.float32) + dt_bias)
    o = gated_delta_rule_chunked(q, k, v, g, beta).astype(u.dtype)
    o = rms_norm(o, norm_g) * jax.nn.silu(z.reshape(B, S, GDN_V_HEADS, GDN_DV))
    return o.reshape(B, S, v_dim) @ w_o


def gla_chunked(q, k, v, log_alpha):
    B, S, H, DK = q.shape
    DV = v.shape[-1]
    C = GLA_CHUNK
    N = S // C
    f32 = jnp.float32

    def chunks(t):
        return t.astype(f32).reshape(B, N, C, H, t.shape[-1]).swapaxes(2, 3)

    q = chunks(q) * (DK ** -0.5)
    k, v, la = chunks(k), chunks(v), chunks(log_alpha)
    b = jnp.cumsum(la, axis=-2)
    b_last = b[..., -1, :]
    q_dec = q * jnp.exp(b)
    k_inv = k * jnp.exp(-b)
    k_tail = k * jnp.exp(b_last[..., None, :] - b)
    causal = jnp.tril(jnp.ones((C, C), bool))
    attn = jnp.where(causal, jnp.einsum('bnhid,bnhjd->bnhij', q_dec, k_inv), 0.0)
    o_intra = jnp.einsum('bnhij,bnhje->bnhie', attn, v)

    def step(state, xs):
        qd, kt, vc, bl = xs
        o_c = jnp.einsum('bhcd,bhde->bhce', qd, state)
        state = state * jnp.exp(bl)[..., None] + jnp.einsum('bhcd,bhce->bhde', kt, vc)
        return state, o_c

    xs = tuple(jnp.moveaxis(t, 1, 0) for t in (q_dec, k_tail, v, b_last))
    _, o_inter = lax.scan(step, jnp.zeros((B, H, DK, DV), f32), xs)
    o = jnp.moveaxis(o_inter, 0, 1) + o_intra
    return o.swapaxes(2, 3).reshape(B, S, H, DV)


def gla_mixer(u, w_in, w_gk, b_gk, norm_g, w_o):
    B, S, _ = u.shape
    H = GLA_HEADS
    dk = GLA_KEY_DIM // H
    dv = GLA_VAL_DIM // H
    proj = u @ w_in
    q, k, v, out_gate, gk_low = jnp.split(
        proj, [GLA_KEY_DIM, 2 * GLA_KEY_DIM, 2 * GLA_KEY_DIM + GLA_VAL_DIM, 2 * GLA_KEY_DIM + 2 * GLA_VAL_DIM], -1)
    log_alpha = jax.nn.log_sigmoid((gk_low @ w_gk + b_gk).astype(jnp.float32)) / GLA_GATE_NORMALIZER
    o = gla_chunked(q.reshape(B, S, H, dk), k.reshape(B, S, H, dk), v.reshape(B, S, H, dv),
                    log_alpha.reshape(B, S, H, dk)).astype(u.dtype)
    o = rms_norm(o, norm_g) * jax.nn.silu(out_gate.reshape(B, S, H, dv))
    return o.reshape(B, S, GLA_VAL_DIM) @ w_o


def moba_mixer(u, pos, w_in, w_o, rel_bias):
    B, S, _ = u.shape
    H, Dh, L = MOBA_HEADS, MOBA_HEAD_DIM, MOBA_BLOCK
    qkv = (u @ w_in).reshape(B, S, 3, H, Dh)
    q = qkv[:, :, 0].transpose(0, 2, 1, 3)
    k = qkv[:, :, 1].transpose(0, 2, 1, 3)
    v = qkv[:, :, 2].transpose(0, 2, 1, 3)
    n_blk = -(-S // L)
    pad = n_blk * L - S
    k_blk = jnp.pad(k, ((0, 0), (0, 0), (0, pad), (0, 0))).reshape(B, H, n_blk, L, Dh)
    v_blk = jnp.pad(v, ((0, 0), (0, 0), (0, pad), (0, 0))).reshape(B, H, n_blk, L, Dh)
    k_mean = jnp.mean(k_blk, axis=-2)
    gate = jnp.einsum('bhsd,bhnd->bhsn', q, k_mean).astype(jnp.float32)
    q_blk = pos // L
    past = jnp.arange(n_blk)[None, :] < q_blk[:, None]
    gate = jnp.where(past, gate, -jnp.inf)
    n_sel = max(min(MOBA_TOPK, n_blk - 1), 1)
    top_val, top_idx = lax.top_k(gate, n_sel)
    own = jnp.broadcast_to(q_blk[None, None, :, None], (B, H, S, 1)).astype(top_idx.dtype)
    idx = jnp.concatenate([top_idx, own], -1)
    valid = jnp.concatenate([jnp.isfinite(top_val), jnp.ones((B, H, S, 1), bool)], -1)
    n_g = n_sel + 1
    nq = S // MOBA_Q_BLOCK

    def split_q(t):
        return jnp.moveaxis(t.reshape(B, H, nq, MOBA_Q_BLOCK, *t.shape[3:]), 2, 0)

    gather = jax.vmap(jax.vmap(lambda blk, ix: blk[ix]))
    scale = Dh ** -0.5
    key_off = jnp.arange(L)
    head_ix = jnp.arange(H)[None, :, None, None, None]
    bias_table = rel_bias.T

    def block(xs):
        qc, ic, vc, pc = xs
        kg = gather(k_blk, ic)
        vg = gather(v_blk, ic)
        logits = jnp.einsum('bhqd,bhqgld->bhqgl', qc, kg).astype(jnp.float32) * scale
        dist = pc[None, None, :, None, None] - (ic[..., None] * L + key_off)
        bias = bias_table[head_ix, t5_bucket(dist)]
        mask = vc[..., None] & (dist >= 0)
        logits = jnp.where(mask, logits + bias, -jnp.inf).reshape(B, H, MOBA_Q_BLOCK, n_g * L)
        p = jax.nn.softmax(logits, -1).reshape(B, H, MOBA_Q_BLOCK, n_g, L).astype(vg.dtype)
        return jnp.einsum('bhqgl,bhqgld->bhqd', p, vg)

    o = lax.map(block, (split_q(q), split_q(idx), split_q(valid), pos.reshape(nq, MOBA_Q_BLOCK)))
    o = o.transpose(1, 0, 3, 2, 4).reshape(B, S, H * Dh)
    return o @ w_o


def moe_ffn(u, router_w, router_b, w_gu, b_gu, w_down, b_down):
    B, S, D = u.shape
    t = u.reshape(B * S, D)
    logits = (t @ router_w + router_b).astype(jnp.float32)
    top_val, top_idx = lax.top_k(logits, TOP_K)
    weights = jax.nn.softmax(top_val, -1)
    gates = jnp.sum(jax.nn.one_hot(top_idx, N_EXPERTS, dtype=jnp.float32) * weights[..., None], axis=1)
    out = jnp.zeros((B * S, D), jnp.float32)
    for e in range(N_EXPERTS):
        gu = t @ w_gu[e] + b_gu[e]
        gl = jnp.minimum(gu[:, :EXPERT_FF], SWIGLU_LIMIT)
        up = jnp.clip(gu[:, EXPERT_FF:], -SWIGLU_LIMIT, SWIGLU_LIMIT)
        h = (up + 1.0) * gl * jax.nn.sigmoid(gl * SWIGLU_ALPHA)
        out = out + gates[:, e:e + 1] * (h @ w_down[e] + b_down[e])
    return out.astype(u.dtype).reshape(B, S, D)


def setup_inputs(seed: int = 0) -> dict:
    key = jax.random.key(seed)
    keys = iter(list(jax.random.split(key, 64)))

    def nrm(shape, scale):
        return jax.random.normal(next(keys), shape, jnp.float32) * scale

    def gain(shape):
        return 1.0 + nrm(shape, 0.02)

    D = D_MODEL
    nA, nB, nC, nD = LAYERS_PER_MIXER
    beta = DEEPNORM_BETA
    mla_in = MLA_Q_LORA + MLA_KV_LORA + MLA_ROPE
    gdn_qk = GDN_K_HEADS * GDN_DK
    gdn_v = GDN_V_HEADS * GDN_DV
    gdn_in = 2 * gdn_qk + 2 * gdn_v + 2 * GDN_V_HEADS
    gdn_conv_ch = 2 * gdn_qk + gdn_v
    gla_in = 2 * GLA_KEY_DIM + 2 * GLA_VAL_DIM + GLA_GATE_RANK
    moba_w = MOBA_HEADS * MOBA_HEAD_DIM
    dt = jnp.exp(jax.random.uniform(next(keys), (nB, GDN_V_HEADS), jnp.float32, math.log(1e-3), math.log(1e-1)))
    a_log = jnp.log(jax.random.uniform(next(keys), (nB, GDN_V_HEADS), jnp.float32, 1.0, 16.0))
    return {
        "x": nrm((BATCH, SEQ, D), 1.0),
        "c": nrm((BATCH, D), 1.0),
        "rel_bias": nrm((REL_BUCKETS, MOBA_HEADS), 0.1),
        "mla_w_in": nrm((nA, D, mla_in), D ** -0.5),
        "mla_q_norm": gain((nA, MLA_Q_LORA)),
        "mla_kv_norm": gain((nA, MLA_KV_LORA)),
        "mla_w_qb": nrm((nA, MLA_Q_LORA, MLA_HEADS * (MLA_NOPE + MLA_ROPE)), MLA_Q_LORA ** -0.5),
        "mla_w_kvb": nrm((nA, MLA_KV_LORA, MLA_HEADS * (MLA_NOPE + MLA_V)), MLA_KV_LORA ** -0.5),
        "mla_w_o": nrm((nA, MLA_HEADS * MLA_V, D), beta * (MLA_HEADS * MLA_V) ** -0.5),
        "gdn_w_in": nrm((nB, D, gdn_in), D ** -0.5),
        "gdn_conv_w": nrm((nB, GDN_CONV, gdn_conv_ch), GDN_CONV ** -0.5),
        "gdn_a_log": a_log,
        "gdn_dt_bias": dt + jnp.log(-jnp.expm1(-dt)),
        "gdn_norm": gain((nB, GDN_DV)),
        "gdn_w_o": nrm((nB, gdn_v, D), beta * gdn_v ** -0.5),
        "gla_w_in": nrm((nC, D, gla_in), D ** -0.5),
        "gla_w_gk": nrm((nC, GLA_GATE_RANK, GLA_KEY_DIM), GLA_GATE_RANK ** -0.5),
        "gla_b_gk": nrm((nC, GLA_KEY_DIM), 0.1),
        "gla_norm": gain((nC, GLA_VAL_DIM // GLA_HEADS)),
        "gla_w_o": nrm((nC, GLA_VAL_DIM, D), beta * GLA_VAL_DIM ** -0.5),
        "moba_w_in": nrm((nD, D, 3 * moba_w), D ** -0.5),
        "moba_w_o": nrm((nD, moba_w, D), beta * moba_w ** -0.5),
        "ada_w": nrm((DEPTH, D, 6 * D), 0.2 * D ** -0.5),
        "ada_b": nrm((DEPTH, 6 * D), 0.02),
        "ln_g": gain((DEPTH, 2, D)),
        "ln_b": nrm((DEPTH, 2, D), 0.02),
        "router_w": nrm((DEPTH, D, N_EXPERTS), D ** -0.5),
        "router_b": nrm((DEPTH, N_EXPERTS), 0.01),
        "moe_w_gu": nrm((DEPTH, N_EXPERTS, D, 2 * EXPERT_FF), D ** -0.5),
        "moe_b_gu": nrm((DEPTH, N_EXPERTS, 2 * EXPERT_FF), 0.02),
        "moe_w_down": nrm((DEPTH, N_EXPERTS, EXPERT_FF, D), beta * EXPERT_FF ** -0.5),
        "moe_b_down": nrm((DEPTH, N_EXPERTS, D), 0.02),
    }


def reference(x, c, rel_bias, mla_w_in, mla_q_norm, mla_kv_norm, mla_w_qb, mla_w_kvb, mla_w_o,
              gdn_w_in, gdn_conv_w, gdn_a_log, gdn_dt_bias, gdn_norm, gdn_w_o,
              gla_w_in, gla_w_gk, gla_b_gk, gla_norm, gla_w_o,
              moba_w_in, moba_w_o, ada_w, ada_b, ln_g, ln_b,
              router_w, router_b, moe_w_gu, moe_b_gu, moe_w_down, moe_b_down):
    B, S, D = x.shape
    pos = jnp.arange(S, dtype=jnp.int32)
    c_act = jax.nn.silu(c)
    for i in range(DEPTH):
        m, j = i % N_MIXERS, i // N_MIXERS
        mod = (c_act @ ada_w[i] + ada_b[i])[:, None, :]
        sh_a, sc_a, g_a, sh_f, sc_f, g_f = jnp.split(mod, 6, -1)
        u = x * (1.0 + sc_a) + sh_a
        if m == 0:
            h = mla_mixer(u, pos, mla_w_in[j], mla_q_norm[j], mla_kv_norm[j], mla_w_qb[j], mla_w_kvb[j], mla_w_o[j])
        elif m == 1:
            h = gdn_mixer(u, gdn_w_in[j], gdn_conv_w[j], gdn_a_log[j], gdn_dt_bias[j], gdn_norm[j], gdn_w_o[j])
        elif m == 2:
            h = gla_mixer(u, gla_w_in[j], gla_w_gk[j], gla_b_gk[j], gla_norm[j], gla_w_o[j])
        else:
            h = moba_mixer(u, pos, moba_w_in[j], moba_w_o[j], rel_bias)
        x = layer_norm(DEEPNORM_ALPHA * x + (1.0 + g_a) * h, ln_g[i, 0], ln_b[i, 0])
        u = x * (1.0 + sc_f) + sh_f
        f = moe_ffn(u, router_w[i], router_b[i], moe_w_gu[i], moe_b_gu[i], moe_w_down[i], moe_b_down[i])
        x = layer_norm(DEEPNORM_ALPHA * x + (1.0 + g_f) * f, ln_g[i, 1], ln_b[i, 1])
    return x
```

```python
import numpy as np
from contextlib import ExitStack
import concourse.bass as bass
import concourse.mybir as mybir
from concourse.bass_utils import run_bass_kernel_spmd

F32 = mybir.dt.float32
BF16 = mybir.dt.bfloat16
I32 = mybir.dt.int32
U32 = mybir.dt.uint32
AF = mybir.ActivationFunctionType
ALU = mybir.AluOpType
AX = mybir.AxisListType

EPOCH = 30000
NDMASEM = 8


class KB:
    def __init__(self, name="k"):
        self.nc = bass.Bass("TRN2", target_bir_lowering=False)
        self.es = ExitStack()
        nc = self.nc
        self.eng = {"pe": nc.tensor, "dve": nc.vector, "act": nc.scalar, "pool": nc.gpsimd, "sp": nc.sync}
        self.sem = {}
        self.cnt = {}
        self.nsem = 0
        for e in self.eng:
            self._new_epoch(e)
        self.dsem = {}
        self.dcnt = {}
        for q in ("sp", "act", "pool"):
            self.dsem[q] = [self.es.enter_context(nc.semaphore(f"d_{q}_{i}")) for i in range(NDMASEM)]
            self.dcnt[q] = 0
        self.waited = {}
        self.lastw = {}
        self.rd = {}
        self.strict_same = True
        self.n_inst = 0

    def _new_epoch(self, e):
        self.nsem += 1
        self.sem[e] = self.es.enter_context(self.nc.semaphore(f"s_{e}_{self.nsem}"))
        self.cnt[e] = 0

    def sb(self, name, shape, dt=F32):
        return self.es.enter_context(self.nc.sbuf_tensor(name, list(shape), dt))

    def ps(self, name, shape, dt=F32):
        return self.es.enter_context(self.nc.psum_tensor(name, list(shape), dt))

    def dram(self, name, shape, dt=F32, kind="ExternalInput"):
        return self.nc.dram_tensor(name, list(shape), dt, kind=kind).ap()

    def _deps(self, e, r, w):
        toks = []
        for k in r:
            t = self.lastw.get(k)
            if t is not None:
                toks.append(t)
            if isinstance(k, tuple) and k[0] == "ps":
                toks.extend(self.rd.get(k, ()))
        for k in w:
            t = self.lastw.get(k)
            if t is not None:
                toks.append(t)
            toks.extend(self.rd.get(k, ()))
        return toks

    def _wait(self, e, toks, pe_skip=False):
        eng = self.eng[e]
        best = {}
        for (s, v, src) in toks:
            if src == e and (not self.strict_same or e == "pe" or e == "sp"):
                continue
            key = id(s)
            if best.get(key, (None, 0))[1] < v:
                best[key] = (s, v)
        for key, (s, v) in best.items():
            if self.waited.get((e, key), 0) >= v:
                continue
            eng.wait_ge(s, v)
            self.waited[(e, key)] = v

    def _record(self, tok, r, w):
        for k in w:
            self.lastw[k] = tok
            self.rd[k] = []
        for k in r:
            self.rd.setdefault(k, []).append(tok)

    def op(self, e, fn, r=(), w=()):
        self._wait(e, self._deps(e, r, w))
        if self.cnt[e] >= EPOCH:
            self._new_epoch(e)
        inst = fn(self.eng[e])
        self.cnt[e] += 1
        inst.then_inc(self.sem[e], 1)
        tok = (self.sem[e], self.cnt[e], e)
        self._record(tok, r, w)
        self.n_inst += 1
        return tok

    def dma(self, q, out, in_, r=(), w=(), **kw):
        i = self.dcnt[q]
        s = self.dsem[q][i % NDMASEM]
        prev = 16 * (i // NDMASEM)
        toks = self._deps(q, r, w)
        if prev > 0:
            toks.append((s, prev, "dma"))
        self._wait(q, toks)
        inst = self.eng[q].dma_start(out=out, in_=in_, **kw)
        inst.then_inc(s, 16)
        self.dcnt[q] += 1
        tok = (s, prev + 16, "dma")
        self._record(tok, r, w)
        self.n_inst += 1
        return tok

    def barrier(self):
        toks = [(self.sem[e], self.cnt[e], "bar") for e in self.eng if self.cnt[e] > 0]
        for q in self.dsem:
            n = self.dcnt[q]
            for j in range(NDMASEM):
                cnt_j = (n - j + NDMASEM - 1) // NDMASEM if n > j else 0
                if cnt_j > 0:
                    toks.append((self.dsem[q][j], 16 * cnt_j, "dma"))
        for e in self.eng:
            self._wait(e, [t for t in toks if not (t[2] == "bar" and t[0] is self.sem[e])])
        self.lastw = {}
        self.rd = {}

    def finish(self, toks):
        self._wait("sp", list(toks))

    def close(self):
        self.es.close()


ALPHA = 8 ** 0.25
LN_EPS = 1e-5
NT = 1024
D = 2048
NE = 32
FF = 768
P_GA, P_SCF, P_SHF, P_GF, P_LG1, P_LB1, P_LG2, P_LB2 = range(8)


def build_B(Kdim, n_exp=NE):
    k = KB()
    nc = k.nc
    KC = Kdim // 128
    oT = k.dram("oT", [Kdim, NT], BF16)
    xT = k.dram("xT", [D, NT])
    w_o = k.dram("w_o", [Kdim, D])
    pp = k.dram("pp", [128, 8, 16])
    rw = k.dram("rw", [D, NE])
    rb = k.dram("rb", [128, NE])
    wgu = k.dram("wgu", [NE, D, 2 * FF])
    bgu = k.dram("bgu", [128, NE, 12])
    wdn = k.dram("wdn", [NE, FF, D])
    bdn = k.dram("bdn", [NE, D])
    outT = k.dram("outT", [D, NT], kind="ExternalOutput")
    nscr = k.dram("nscr", [D, NT], kind="Internal")

    AR = k.sb("arena", [128, 19968])
    accT = k.sb("accT", [128, 16, NT])
    u2T = k.sb("u2T", [128, 16, NT], BF16)
    ident = k.sb("ident", [128, 128])
    ones = k.sb("ones", [128, 128])
    pps = k.sb("pps", [128, 8, 16])
    cf = k.sb("cf", [128, 6, 16])
    rws = k.sb("rws", [128, 16, NE])
    rbs = k.sb("rbs", [128, NE])
    bgs = k.sb("bgs", [128, NE, 12])
    gates = k.sb("gates", [128, 8, NE])
    gT = k.sb("gT", [NE, NT])
    bds = k.sb("bds", [NE, D])
    sel = [k.sb(f"sel{i}", [NE, 128]) for i in range(2)]
    lg = k.sb("lg", [128, NE])
    m8 = k.sb("m8", [128, 8])
    msk = k.sb("msk", [128, NE])
    ssum = k.sb("ssum", [128, 1])
    nm = k.sb("nm", [128, 1])
    PS = [k.ps(f"ps{i}", [128, 512]) for i in range(8)]

    def arena(off, n, dt=F32):
        if dt == F32:
            return AR[:, off:off + n]
        return AR[:, off:off + n // 2].bitcast(BF16)

    k.op("pool", lambda e: e.memset(ident[:], 1.0), w=["ident"])
    k.op("pool", lambda e: e.affine_select(out=ident[:], in_=ident[:], pattern=[[-1, 128]], compare_op=ALU.is_equal,
                                           fill=0.0, base=0, channel_multiplier=1), r=["ident"], w=["ident"])
    k.op("pool", lambda e: e.memset(ones[:], 1.0), w=["ones"])
    k.dma("sp", pps[:], pp[:, :, :], w=["pps"])
    k.dma("sp", rws[:], rw.rearrange("(c p) e -> p c e", p=128), w=["rws"])
    k.dma("sp", rbs[:], rb[:, :], w=["rbs"])
    k.dma("sp", bgs[:], bgu[:, :, :], w=["bgs"])
    k.dma("sp", bds[:], bdn[:, :], w=["bds"])
    k.op("dve", lambda e: e.tensor_scalar_add(out=bgs[:, :, 6:12], in0=bgs[:, :, 6:12], scalar1=1.0), r=["bgs"], w=["bgs"])
    G1, A_, B_, G2, aLG1, aLB1 = (cf[:, i, :] for i in range(6))
    k.op("dve", lambda e: e.tensor_scalar_add(out=G1, in0=pps[:, P_GA, :], scalar1=1.0), r=["pps"], w=["cf0"])
    k.op("dve", lambda e: e.tensor_scalar_add(out=G2, in0=pps[:, P_GF, :], scalar1=1.0), r=["pps"], w=["cf3"])
    k.op("dve", lambda e: e.tensor_scalar_add(out=B_, in0=pps[:, P_SCF, :], scalar1=1.0), r=["pps"], w=["cf2"])
    k.op("dve", lambda e: e.tensor_tensor(out=A_, in0=pps[:, P_LG1, :], in1=B_, op=ALU.mult), r=["pps", "cf2"], w=["cf1"])
    k.op("dve", lambda e: e.tensor_tensor(out=B_, in0=pps[:, P_LB1, :], in1=B_, op=ALU.mult), r=["pps", "cf2"], w=["cf2"])
    k.op("dve", lambda e: e.tensor_tensor(out=B_, in0=B_, in1=pps[:, P_SHF, :], op=ALU.add), r=["pps", "cf2"], w=["cf2"])
    k.op("dve", lambda e: e.tensor_scalar_mul(out=aLG1, in0=pps[:, P_LG1, :], scalar1=ALPHA), r=["pps"], w=["cf4"])
    k.op("dve", lambda e: e.tensor_scalar_mul(out=aLB1, in0=pps[:, P_LB1, :], scalar1=ALPHA), r=["pps"], w=["cf5"])
    CF = ["cf0", "cf1", "cf2", "cf3", "cf4", "cf5"]

    def ln_stats_finish(ps_s, ps_q, keys):
        for th in range(2):
            sl = slice(th * 512, (th + 1) * 512)
            k.op("act", lambda e: e.activation(out=mu[:, sl], in_=PS[ps_s[th]][:], func=AF.Copy, scale=1.0 / D), r=[("ps", ps_s[th])], w=[("mu", th)])
            k.op("dve", lambda e: e.tensor_tensor(out=rs[:, sl], in0=mu[:, sl], in1=mu[:, sl], op=ALU.mult), r=[("mu", th)], w=[("rs", th)])
            k.op("dve", lambda e: e.scalar_tensor_tensor(out=rs[:, sl], in0=PS[ps_q[th]][:], scalar=1.0 / D, in1=rs[:, sl], op0=ALU.mult, op1=ALU.subtract),
                 r=[("ps", ps_q[th]), ("rs", th)], w=[("rs", th)])
            k.op("dve", lambda e: e.tensor_scalar_add(out=rs[:, sl], in0=rs[:, sl], scalar1=LN_EPS), r=[("rs", th)], w=[("rs", th)])
            k.op("act", lambda e: e.activation(out=rs[:, sl], in_=rs[:, sl], func=AF.Sqrt), r=[("rs", th)], w=[("rs", th)])
            k.op("dve", lambda e: e.reciprocal(out=rs[:, sl], in_=rs[:, sl]), r=[("rs", th)], w=[("rs", th)])

    o_oT = 0
    o_wo = o_oT + 8192
    o_x = o_wo + 2 * KC * 64
    o_sq = o_x + 2 * 1024
    o_u32 = o_sq + 2 * 512
    o_mu = o_u32 + 2 * 512
    o_rs = o_mu + 1024
    o_lT = o_rs + 1024
    assert o_lT + 1024 <= 19968, o_lT
    mu = arena(o_mu, 1024)
    rs = arena(o_rs, 1024)
    lT = arena(o_lT, 1024)[0:NE, :]
    oT_lo = arena(o_oT, 16 * 1024, BF16).rearrange("p (c n) -> p c n", c=16)
    k.dma("sp", oT_lo[:], oT[0:2048, :].rearrange("(c p) t -> p c t", p=128), w=["oTs"])
    if KC > 16:
        k.dma("sp", u2T[:], oT[2048:4096, :].rearrange("(c p) t -> p c t", p=128), w=["oTs2"])

    def oT_s_(c, sl):
        return oT_lo[:, c, sl] if c < 16 else u2T[:, c - 16, sl]
    for nb in range(16):
        wo_b = arena(o_wo + (nb % 2) * KC * 64, KC * 128, BF16).rearrange("p (c n) -> p c n", c=KC)
        xb = arena(o_x + (nb % 2) * 1024, 1024)
        kw, kx = ("wo", nb % 2), ("xb", nb % 2)
        k.dma("pool", wo_b[:], w_o[:, nb * 128:(nb + 1) * 128].rearrange("(c p) n -> p c n", p=128), w=[kw])
        k.dma("sp", xb, xT[nb * 128:(nb + 1) * 128, :], w=[kx])
        k.op("act", lambda e: e.activation(out=xb, in_=xb, func=AF.Copy, scale=ALPHA), r=[kx], w=[kx])
        for th in range(2):
            sl = slice(th * 512, (th + 1) * 512)
            ph = PS[th]
            for c in range(KC):
                k.op("pe", lambda e: e.matmul(ph[:], lhsT=wo_b[:, c, :], rhs=oT_s_(c, sl), start=(c == 0), stop=(c == KC - 1)),
                     r=["oTs", "oTs2", kw], w=[("ps", th)])
            ky = ("yT", nb, th)
            k.op("dve", lambda e: e.scalar_tensor_tensor(out=accT[:, nb, sl], in0=ph[:], scalar=G1[:, nb:nb + 1], in1=xb[:, sl], op0=ALU.mult, op1=ALU.add),
                 r=[("ps", th), kx, "cf0"], w=[ky])
            sq = arena(o_sq + th * 512, 512)
            k.op("act", lambda e: e.activation(out=sq, in_=accT[:, nb, sl], func=AF.Square), r=[ky], w=[("sq", th)])
            k.op("pe", lambda e: e.matmul(PS[2 + th][:], lhsT=ones[:], rhs=accT[:, nb, sl], start=(nb == 0), stop=(nb == 15)), r=[ky, "ones"], w=[("ps", 2 + th)])
            k.op("pe", lambda e: e.matmul(PS[4 + th][:], lhsT=ones[:], rhs=sq, start=(nb == 0), stop=(nb == 15)), r=[("sq", th), "ones"], w=[("ps", 4 + th)])
    ln_stats_finish([2, 3], [4, 5], None)
    for th in range(2):
        sl = slice(th * 512, (th + 1) * 512)
        for fc in range(16):
            ky = ("yT", fc, th)
            y = accT[:, fc, sl]
            k.op("dve", lambda e: e.tensor_tensor(out=y, in0=y, in1=mu[:, sl], op=ALU.subtract), r=[ky, ("mu", th)], w=[ky])
            k.op("dve", lambda e: e.tensor_tensor(out=y, in0=y, in1=rs[:, sl], op=ALU.mult), r=[ky, ("rs", th)], w=[ky])
            k.dma("sp", nscr[fc * 128:(fc + 1) * 128, sl], y, r=[ky], w=[("nscr", fc, th)])
            k.op("act", lambda e: e.activation(out=u2T[:, fc, sl], in_=y, func=AF.Identity, scale=A_[:, fc:fc + 1], bias=B_[:, fc:fc + 1]),
                 r=[ky, "cf1", "cf2"], w=[("u2T", fc, th), "oTs2"])
            u32 = arena(o_u32 + (fc % 2) * 512, 512)
            ku = ("u32", fc % 2)
            k.op("act", lambda e: e.activation(out=u32, in_=y, func=AF.Identity, scale=A_[:, fc:fc + 1], bias=B_[:, fc:fc + 1]),
                 r=[ky, "cf1", "cf2"], w=[ku])
            k.op("pe", lambda e: e.matmul(PS[6 + th][0:NE, :], lhsT=rws[:, fc, :], rhs=u32, start=(fc == 0), stop=(fc == 15)), r=[ku, "rws"], w=[("ps", 6 + th)])
        k.op("act", lambda e: e.copy(out=lT[:, sl], in_=PS[6 + th][0:NE, :]), r=[("ps", 6 + th)], w=[("lT", th)])
    for tt in range(8):
        pi = tt % 2
        k.op("pe", lambda e: e.transpose(PS[pi][:, 0:NE], lT[:, tt * 128:(tt + 1) * 128], ident[0:NE, 0:NE]), r=[("lT", tt // 4), "ident"], w=[("ps", pi)])
        k.op("dve", lambda e: e.tensor_tensor(out=lg[:], in0=PS[pi][:, 0:NE], in1=rbs[:], op=ALU.add), r=[("ps", pi), "rbs"], w=["lg"])
        k.op("dve", lambda e: e.max(out=m8[:], in_=lg[:]), r=["lg"], w=["m8"])
        k.op("dve", lambda e: e.tensor_scalar(out=msk[:], in0=lg[:], scalar1=m8[:, 3:4], scalar2=None, op0=ALU.is_ge), r=["lg", "m8"], w=["msk"])
        k.op("dve", lambda e: e.tensor_scalar_mul(out=nm[:], in0=m8[:, 0:1], scalar1=-1.0), r=["m8"], w=["nm"])
        k.op("act", lambda e: e.activation(out=lg[:], in_=lg[:], func=AF.Exp, bias=nm[:, 0:1], scale=1.0), r=["lg", "nm"], w=["lg"])
        k.op("dve", lambda e: e.tensor_tensor(out=lg[:], in0=lg[:], in1=msk[:], op=ALU.mult), r=["lg", "msk"], w=["lg"])
        k.op("dve", lambda e: e.reduce_sum(out=ssum[:], in_=lg[:], axis=AX.X), r=["lg"], w=["ssum"])
        k.op("dve", lambda e: e.reciprocal(out=ssum[:], in_=ssum[:]), r=["ssum"], w=["ssum"])
        k.op("dve", lambda e: e.tensor_scalar_mul(out=gates[:, tt, :], in0=lg[:], scalar1=ssum[:, 0:1]), r=["lg", "ssum"], w=[("gates", tt)])
        k.op("pe", lambda e: e.transpose(PS[2 + pi][0:NE, 0:128], gates[:, tt, :], ident[:]), r=[("gates", tt), "ident"], w=[("ps", 2 + pi)])
        k.op("act", lambda e: e.copy(out=gT[:, tt * 128:(tt + 1) * 128], in_=PS[2 + pi][0:NE, 0:128]), r=[("ps", 2 + pi)], w=[("gT", tt // 4)])

    k.barrier()
    o_wgu = 0
    o_hT = o_wgu + 2 * 2048
    o_wd = o_hT + 2 * 3072
    o_tmp = o_wd + 3 * 1536
    o_gb = o_tmp + 6 * 512
    assert o_gb + 2 * 1024 <= 19968, o_gb
    wg_b = [arena(o_wgu + i * 2048, 4096, BF16).rearrange("p (c n) -> p c n", c=16) for i in range(2)]
    hT_b = [arena(o_hT + i * 3072, 6144, BF16).rearrange("p (c n) -> p c n", c=6) for i in range(2)]
    wd_b = [arena(o_wd + i * 1536, 3072, BF16).rearrange("p (c n) -> p c n", c=6) for i in range(3)]
    tmp = [[arena(o_tmp + (b * 3 + i) * 512, 512) for i in range(3)] for b in range(2)]
    Gb = [arena(o_gb + i * 1024, 1024) for i in range(2)]

    for nb in range(16):
        for th in range(2):
            sl = slice(th * 512, (th + 1) * 512)
            pi = (nb * 2 + th) % 4
            k.op("pe", lambda e: e.matmul(PS[pi][:], lhsT=bds[:, nb * 128:(nb + 1) * 128], rhs=gT[:, sl], start=True, stop=True),
                 r=[("gT", th), "bds"], w=[("ps", pi)])
            k.op("act", lambda e: e.copy(out=accT[:, nb, sl], in_=PS[pi][:]), r=[("ps", pi)], w=[("acc", nb, th)])

    gcount = [0]

    def load_group(e_, j):
        g = gcount[0]; gcount[0] += 1
        b = wg_b[g % 2]
        for h in range(2):
            c0 = h * FF + j * 128
            k.dma("pool", b[:, :, h * 128:(h + 1) * 128], wgu[e_, :, c0:c0 + 128].rearrange("(c p) n -> p c n", p=128), w=[("wg", g % 2, h)])
        return g % 2

    wcount = [0]

    def load_wd(e_, q):
        g = wcount[0]; wcount[0] += 1
        b = wd_b[g % 3]
        k.dma("pool", b[:], wdn[e_, :, q * 512:(q + 1) * 512].rearrange("(c p) n -> p c n", p=128), w=[("wd", g % 3)])
        return g % 3

    it = [0]

    def bcast_gate(e_):
        s = sel[e_ % 2]
        ks = ("sel", e_ % 2)
        k.op("pool", lambda e: e.memset(s[:], 1.0), w=[ks])
        k.op("pool", lambda e: e.affine_select(out=s[:], in_=s[:], pattern=[[0, 128]], compare_op=ALU.is_equal, fill=0.0,
                                               base=-e_, channel_multiplier=1), r=[ks], w=[ks])
        for th in range(2):
            sl = slice(th * 512, (th + 1) * 512)
            k.op("pe", lambda e: e.matmul(PS[6 + th][:], lhsT=s[:], rhs=gT[:, sl], start=True, stop=True), r=[ks, ("gT", th)], w=[("ps", 6 + th)])
            k.op("act", lambda e: e.copy(out=Gb[e_ % 2][:, sl], in_=PS[6 + th][:]), r=[("ps", 6 + th)], w=[("Gb", e_ % 2, th)])

    def gu_phase(e_, pre):
        slots = list(pre)
        for j in range(6):
            if j + 1 < 6:
                slots.append(load_group(e_, j + 1))
            elif e_ + 1 < n_exp:
                nxt.append(load_group(e_ + 1, 0))
            s = slots[j]
            b = wg_b[s]
            for th in range(2):
                sl = slice(th * 512, (th + 1) * 512)
                i = it[0]; it[0] += 1
                pg, pu = PS[(i % 2) * 2], PS[(i % 2) * 2 + 1]
                kg, ku = ("ps", (i % 2) * 2), ("ps", (i % 2) * 2 + 1)
                ru = [("u2T", c, th) for c in range(16)]
                for c in range(16):
                    k.op("pe", lambda e: e.matmul(pg[:], lhsT=b[:, c, 0:128], rhs=u2T[:, c, sl], start=(c == 0), stop=(c == 15)),
                         r=[("wg", s, 0)] + ru, w=[kg])
                for c in range(16):
                    k.op("pe", lambda e: e.matmul(pu[:], lhsT=b[:, c, 128:256], rhs=u2T[:, c, sl], start=(c == 0), stop=(c == 15)),
                         r=[("wg", s, 1)] + ru, w=[ku])
                gl, sg, up = tmp[i % 2]
                kt = ("tmp", i % 2)
                k.op("dve", lambda e: e.tensor_scalar(out=gl, in0=pg[:], scalar1=bgs[:, e_, j:j + 1], scalar2=7.0, op0=ALU.add, op1=ALU.min),
                     r=[kg, "bgs"], w=[kt + ("gl",)])
                k.op("act", lambda e: e.activation(out=sg, in_=gl, func=AF.Sigmoid, scale=1.702), r=[kt + ("gl",)], w=[kt + ("sg",)])
                k.op("dve", lambda e: e.tensor_scalar(out=up, in0=pu[:], scalar1=bgs[:, e_, 6 + j:7 + j], scalar2=8.0, op0=ALU.add, op1=ALU.min),
                     r=[ku, "bgs"], w=[kt + ("up",)])
                k.op("dve", lambda e: e.scalar_tensor_tensor(out=up, in0=up, scalar=-6.0, in1=gl, op0=ALU.max, op1=ALU.mult),
                     r=[kt + ("up",), kt + ("gl",)], w=[kt + ("up",)])
                k.op("dve", lambda e: e.tensor_tensor(out=sg, in0=sg, in1=Gb[e_ % 2][:, sl], op=ALU.mult), r=[kt + ("sg",), ("Gb", e_ % 2, th)], w=[kt + ("sg",)])
                k.op("dve", lambda e: e.tensor_tensor(out=hT_b[e_ % 2][:, j, sl], in0=up, in1=sg, op=ALU.mult),
                     r=[kt + ("up",), kt + ("sg",)], w=[("hT", e_ % 2, j, th)])

    def down_phase(e_, pre_wd):
        hb = hT_b[e_ % 2]
        wslots = list(pre_wd)
        for q in range(4):
            if q + 2 < 4:
                wslots.append(load_wd(e_, q + 2))
            elif e_ + 1 < n_exp:
                nxt_wd.append(load_wd(e_ + 1, q + 2 - 4))
            ws = wslots[q]
            wb = wd_b[ws]
            for nbq in range(4):
                nb = q * 4 + nbq
                for th in range(2):
                    sl = slice(th * 512, (th + 1) * 512)
                    i = it[0]; it[0] += 1
                    pi = 4 + (i % 2)
                    for c in range(6):
                        k.op("pe", lambda e: e.matmul(PS[pi][:], lhsT=wb[:, c, nbq * 128:(nbq + 1) * 128], rhs=hb[:, c, sl], start=(c == 0), stop=(c == 5)),
                             r=[("hT", e_ % 2, c, th), ("wd", ws)], w=[("ps", pi)])
                    a = accT[:, nb, sl]
                    k.op("dve", lambda e: e.tensor_tensor(out=a, in0=PS[pi][:], in1=a, op=ALU.add), r=[("ps", pi), ("acc", nb, th)], w=[("acc", nb, th)])

    nxt = [load_group(0, 0)]
    nxt_wd = [load_wd(0, 0), load_wd(0, 1)]
    bcast_gate(0)
    for e_ in range(n_exp):
        pre = nxt
        nxt = []
        if e_ + 1 < n_exp:
            bcast_gate(e_ + 1)
        gu_phase(e_, pre)
        if e_ >= 1:
            pw = nxt_wd
            nxt_wd = []
            down_phase(e_ - 1, pw)
    pw = nxt_wd
    nxt_wd = []
    down_phase(n_exp - 1, pw)

    k.barrier()
    o_n = 0
    o_s = 1024
    mu = arena(2048, 1024)
    rs = arena(3072, 1024)
    for fc in range(16):
        for th in range(2):
            sl = slice(th * 512, (th + 1) * 512)
            i = fc * 2 + th
            nt = arena(o_n + (i % 2) * 512, 512)
            kn = ("nt", i % 2)
            k.dma("sp", nt, nscr[fc * 128:(fc + 1) * 128, sl], w=[kn])
            k.op("act", lambda e: e.activation(out=nt, in_=nt, func=AF.Identity, scale=aLG1[:, fc:fc + 1], bias=aLB1[:, fc:fc + 1]), r=[kn] + CF, w=[kn])
            a = accT[:, fc, sl]
            ka = ("acc", fc, th)
            k.op("dve", lambda e: e.scalar_tensor_tensor(out=a, in0=a, scalar=G2[:, fc:fc + 1], in1=nt, op0=ALU.mult, op1=ALU.add), r=[ka, kn] + CF, w=[ka])
            sq = arena(o_s + (i % 2) * 512, 512)
            k.op("act", lambda e: e.activation(out=sq, in_=a, func=AF.Square), r=[ka], w=[("sq", i % 2)])
            k.op("pe", lambda e: e.matmul(PS[th][:], lhsT=ones[:], rhs=a, start=(fc == 0), stop=(fc == 15)), r=[ka, "ones"], w=[("ps", th)])
            k.op("pe", lambda e: e.matmul(PS[2 + th][:], lhsT=ones[:], rhs=sq, start=(fc == 0), stop=(fc == 15)), r=[("sq", i % 2), "ones"], w=[("ps", 2 + th)])
    ln_stats_finish([0, 1], [2, 3], None)
    outs = []
    for fc in range(16):
        for th in range(2):
            sl = slice(th * 512, (th + 1) * 512)
            a = accT[:, fc, sl]
            ka = ("acc", fc, th)
            k.op("dve", lambda e: e.tensor_tensor(out=a, in0=a, in1=mu[:, sl], op=ALU.subtract), r=[ka, ("mu", th)], w=[ka])
            k.op("dve", lambda e: e.tensor_tensor(out=a, in0=a, in1=rs[:, sl], op=ALU.mult), r=[ka, ("rs", th)], w=[ka])
            k.op("act", lambda e: e.activation(out=a, in_=a, func=AF.Identity, scale=pps[:, P_LG2, fc:fc + 1], bias=pps[:, P_LB2, fc:fc + 1]), r=[ka, "pps"], w=[ka])
        outs.append(k.dma("sp", outT[fc * 128:(fc + 1) * 128, :], accT[:, fc, :], r=[("acc", fc, 0), ("acc", fc, 1)]))
    k.finish(outs)
    k.close()
    return nc


MLA_S = 2048
MLA_D = 2048
MLA_RMS_EPS = 1e-6
MLA_NH = 8
MLA_SCALE = 192 ** -0.5


def build_A_mla(nh=MLA_NH, nqt=16):
    k = KB()
    nc = k.nc
    xT = k.dram("xT", [MLA_D, MLA_S])
    pp = k.dram("pp", [128, 2, 16])
    w_in = k.dram("w_in", [MLA_D, 1088])
    nrm = k.dram("nrm", [128, 2, 4])
    w_qb = k.dram("w_qb", [512, MLA_NH * 192])
    w_kvb = k.dram("w_kvb", [512, MLA_NH * 256])
    cs = k.dram("cs", [32, 2, MLA_S])
    cmask = k.dram("cmask", [128, 128])
    oT = k.dram("oT", [MLA_NH * 128, MLA_S], BF16, kind="ExternalOutput")

    AR = k.sb("arena", [128, 29184])
    latn = k.sb("latn", [128, 8, MLA_S], BF16)
    kr = k.sb("kr", [32, 2, MLA_S], BF16)
    css = k.sb("css", [32, 2, MLA_S])
    pps = k.sb("pps", [128, 2, 16])
    nrs = k.sb("nrs", [128, 2, 4])
    ident = k.sb("ident", [128, 128])
    ones = k.sb("ones", [128, 128])
    cm = k.sb("cm", [128, 128])
    m4 = k.sb("m4", [128, 4])
    mx = k.sb("mx", [128, 1])
    rsum = k.sb("rsum", [128, 5])
    rinv = k.sb("rinv", [128, 1])
    dtile = k.sb("dtile", [128, 128])
    dg = k.sb("dg", [128, 128], BF16)
    rt = [k.sb(f"rt{i}", [32, 512]) for i in range(4)]
    PS = [k.ps(f"ps{i}", [128, 512]) for i in range(8)]

    def arena(off, n, dt=F32):
        if dt == F32:
            return AR[:, off:off + n]
        return AR[:, off:off + n // 2].bitcast(BF16)

    k.op("pool", lambda e: e.memset(ident[:], 1.0), w=["ident"])
    k.op("pool", lambda e: e.affine_select(out=ident[:], in_=ident[:], pattern=[[-1, 128]], compare_op=ALU.is_equal,
                                           fill=0.0, base=0, channel_multiplier=1), r=["ident"], w=["ident"])
    k.op("pool", lambda e: e.memset(ones[:], 1.0), w=["ones"])
    k.dma("sp", pps[:], pp[:, :, :], w=["pps"])
    k.dma("sp", nrs[:], nrm[:, :, :], w=["nrs"])
    k.dma("sp", css[:], cs[:, :, :], w=["css"])
    k.dma("sp", cm[:], cmask[:, :], w=["cm"])
    k.op("dve", lambda e: e.tensor_scalar_add(out=pps[:, 0, :], in0=pps[:, 0, :], scalar1=1.0), r=["pps"], w=["pps"])

    o_u = 0
    o_w = 16384
    o_x = o_w + 8704
    assert o_x + 4096 <= 29184
    uT = arena(o_u, 16 * MLA_S, BF16).rearrange("p (c n) -> p c n", c=16)
    wi = arena(o_w, 16 * 1088, BF16).rearrange("p (c n) -> p c n", c=16)
    for c in range(16):
        k.dma("pool", wi[:, c, :], w_in[c * 128:(c + 1) * 128, :], w=[("wi", c)])
    for c in range(16):
        xb = arena(o_x + (c % 2) * 2048, 2048)
        k.dma("sp", xb, xT[c * 128:(c + 1) * 128, :], w=[("xb", c % 2)])
        k.op("act", lambda e: e.activation(out=uT[:, c, :], in_=xb, func=AF.Identity, scale=pps[:, 0, c:c + 1], bias=pps[:, 1, c:c + 1]),
             r=[("xb", c % 2), "pps"], w=[("uT", c)])
    RU = [("uT", c) for c in range(16)]
    RW = [("wi", c) for c in range(16)]

    def rope(x1, x2, o1, o2, tsl, rk, wk, scale=1.0):
        cos, sin = css[:, 0, tsl], css[:, 1, tsl]
        w_ = tsl.stop - tsl.start
        a, b_, c_, d_ = (rt[i][:, 0:w_] for i in range(4))
        k.op("dve", lambda e: e.tensor_tensor(out=a, in0=x1, in1=cos, op=ALU.mult), r=rk + ["css"], w=["rt0"])
        k.op("dve", lambda e: e.tensor_tensor(out=b_, in0=x2, in1=sin, op=ALU.mult), r=rk + ["css"], w=["rt1"])
        k.op("dve", lambda e: e.tensor_tensor(out=c_, in0=x2, in1=cos, op=ALU.mult), r=rk + ["css"], w=["rt2"])
        k.op("dve", lambda e: e.tensor_tensor(out=d_, in0=x1, in1=sin, op=ALU.mult), r=rk + ["css"], w=["rt3"])
        if scale == 1.0:
            k.op("dve", lambda e: e.tensor_tensor(out=o1, in0=a, in1=b_, op=ALU.subtract), r=["rt0", "rt1"], w=wk)
            k.op("dve", lambda e: e.tensor_tensor(out=o2, in0=c_, in1=d_, op=ALU.add), r=["rt2", "rt3"], w=wk)
        else:
            k.op("dve", lambda e: e.tensor_tensor(out=a, in0=a, in1=b_, op=ALU.subtract), r=["rt0", "rt1"], w=["rt0"])
            k.op("dve", lambda e: e.tensor_tensor(out=c_, in0=c_, in1=d_, op=ALU.add), r=["rt2", "rt3"], w=["rt2"])
            k.op("act", lambda e: e.activation(out=o1, in_=a, func=AF.Copy, scale=scale), r=["rt0"], w=wk)
            k.op("act", lambda e: e.activation(out=o2, in_=c_, func=AF.Copy, scale=scale), r=["rt2"], w=wk)

    sqb = [k.sb(f"sqb{i}", [128, 512]) for i in range(2)]
    rstd = k.sb("rstd", [128, 512])
    for tc_ in range(4):
        tsl = slice(tc_ * 512, (tc_ + 1) * 512)
        for grp in range(2):
            for blk in range(4):
                col0 = grp * 512 + blk * 128
                for c in range(16):
                    k.op("pe", lambda e: e.matmul(PS[blk][:], lhsT=wi[:, c, col0:col0 + 128], rhs=uT[:, c, tsl], start=(c == 0), stop=(c == 15)),
                         r=[("uT", c), ("wi", c)], w=[("ps", blk)])
                sq = sqb[blk % 2]
                k.op("act", lambda e: e.activation(out=sq[:], in_=PS[blk][:], func=AF.Square), r=[("ps", blk)], w=[("sq", blk % 2)])
                k.op("pe", lambda e: e.matmul(PS[4][:], lhsT=ones[:], rhs=sq[:], start=(blk == 0), stop=(blk == 3)), r=[("sq", blk % 2), "ones"], w=[("ps", 4)])
            k.op("dve", lambda e: e.tensor_scalar(out=rstd[:], in0=PS[4][:], scalar1=1.0 / 512, scalar2=MLA_RMS_EPS, op0=ALU.mult, op1=ALU.add), r=[("ps", 4)], w=["rstd"])
            k.op("act", lambda e: e.activation(out=rstd[:], in_=rstd[:], func=AF.Sqrt), r=["rstd"], w=["rstd"])
            k.op("dve", lambda e: e.reciprocal(out=rstd[:], in_=rstd[:]), r=["rstd"], w=["rstd"])
            for blk in range(4):
                k.op("dve", lambda e: e.scalar_tensor_tensor(out=latn[:, grp * 4 + blk, tsl], in0=PS[blk][:], scalar=nrs[:, grp, blk:blk + 1], in1=rstd[:],
                                                             op0=ALU.mult, op1=ALU.mult), r=[("ps", blk), "nrs", "rstd"], w=[("latn", grp * 4 + blk, tc_)])
        for hf in range(2):
            col0 = 1024 + hf * 32
            for c in range(16):
                k.op("pe", lambda e: e.matmul(PS[5 + hf][0:32, :], lhsT=wi[:, c, col0:col0 + 32], rhs=uT[:, c, tsl], start=(c == 0), stop=(c == 15)),
                     r=[("uT", c), ("wi", c)], w=[("ps", 5 + hf)])
        rope(PS[5][0:32, :], PS[6][0:32, :], kr[:, 0, tsl], kr[:, 1, tsl], tsl, [("ps", 5), ("ps", 6)], [("kr", tc_)])

    k.barrier()
    o_wq = 0
    o_wk = o_wq + 4 * MLA_NH * 96
    o_hd = o_wk + 4 * MLA_NH * 128
    o_P = o_hd + 2 * 5120
    o_PT = o_P + 2 * 1024
    o_o = o_PT + 2 * 256
    assert o_o + 2048 <= 29184, o_o
    wq = arena(o_wq, 4 * MLA_NH * 192, BF16).rearrange("p (c n) -> p c n", c=4)
    wk = arena(o_wk, 4 * MLA_NH * 256, BF16).rearrange("p (c n) -> p c n", c=4)
    for c in range(4):
        k.dma("pool", wq[:, c, :], w_qb[c * 128:(c + 1) * 128, :], w=[("wq", c)])
        k.dma("pool", wk[:, c, :], w_kvb[c * 128:(c + 1) * 128, :], w=[("wk", c)])
    RQ = [("wq", c) for c in range(4)]
    RK = [("wk", c) for c in range(4)]
    outs = []
    for h in range(nh):
        hb = h % 2
        base = o_hd + hb * 5120
        qn = arena(base, 2048, BF16)
        kn = arena(base + 1024, 2048, BF16)
        V = arena(base + 2048, 2048, BF16).rearrange("p (t d) -> p t d", t=16)
        qr = arena(base + 3072, 2 * 2048, BF16)[0:32, :].rearrange("p (a t) -> p a t", a=2)
        osb = arena(o_o + hb * 1024, 2048, BF16)
        KH = ("hd", hb)
        for tc_ in range(4):
            tsl = slice(tc_ * 512, (tc_ + 1) * 512)
            LQ = [("latn", c, tc_) for c in range(4)]
            LK = [("latn", 4 + c, tc_) for c in range(4)]
            for c in range(4):
                k.op("pe", lambda e: e.matmul(PS[0][:], lhsT=wq[:, c, h * 192:h * 192 + 128], rhs=latn[:, c, tsl], start=(c == 0), stop=(c == 3)), r=RQ + LQ, w=[("ps", 0)])
            k.op("act", lambda e: e.activation(out=qn[:, tsl], in_=PS[0][:], func=AF.Copy, scale=MLA_SCALE), r=[("ps", 0)], w=[KH + ("qn", tc_)])
            for hf in range(2):
                c0 = h * 192 + 128 + hf * 32
                for c in range(4):
                    k.op("pe", lambda e: e.matmul(PS[1 + hf][0:32, :], lhsT=wq[:, c, c0:c0 + 32], rhs=latn[:, c, tsl], start=(c == 0), stop=(c == 3)), r=RQ + LQ, w=[("ps", 1 + hf)])
            rope(PS[1][0:32, :], PS[2][0:32, :], qr[:, 0, tsl], qr[:, 1, tsl], tsl, [("ps", 1), ("ps", 2)], [KH + ("qr", tc_)], scale=MLA_SCALE)
            for c in range(4):
                k.op("pe", lambda e: e.matmul(PS[3][:], lhsT=wk[:, c, h * 256:h * 256 + 128], rhs=latn[:, 4 + c, tsl], start=(c == 0), stop=(c == 3)), r=RK + LK, w=[("ps", 3)])
            k.op("act", lambda e: e.copy(out=kn[:, tsl], in_=PS[3][:]), r=[("ps", 3)], w=[KH + ("kn", tc_)])
            for t4 in range(4):
                tt = tc_ * 4 + t4
                for c in range(4):
                    k.op("pe", lambda e: e.matmul(PS[4][:, t4 * 128:(t4 + 1) * 128], lhsT=latn[:, 4 + c, tt * 128:(tt + 1) * 128],
                                                  rhs=wk[:, c, h * 256 + 128:h * 256 + 256], start=(c == 0), stop=(c == 3)), r=RK + LK, w=[("ps", 4)])
            k.op("act", lambda e: e.copy(out=V[:, tc_ * 4:(tc_ + 1) * 4, :], in_=PS[4][:].rearrange("p (t d) -> p t d", t=4)), r=[("ps", 4)], w=[KH + ("V", tc_)])
        for qt in range(nqt):
            qsl = slice(qt * 128, (qt + 1) * 128)
            nk = (qt + 1) * 128
            nch = (nk + 511) // 512
            pb = (h * nqt + qt) % 2
            P = arena(o_P + pb * 1024, 2048, BF16)
            KP = ("P", pb)
            rq = [KH + ("qn", qt // 4), KH + ("qr", qt // 4)]
            for c in range(nch):
                w_ = min(512, nk - c * 512)
                ksl = slice(c * 512, c * 512 + w_)
                rk_ = rq + [KH + ("kn", c), ("kr", c)]
                k.op("pe", lambda e: e.matmul(PS[c][:, 0:w_], lhsT=qn[:, qsl], rhs=kn[:, ksl], start=True, stop=False), r=rk_, w=[("ps", c)])
                k.op("pe", lambda e: e.matmul(PS[c][:, 0:w_], lhsT=qr[:, 0, qsl], rhs=kr[:, 0, ksl], start=False, stop=False), r=rk_, w=[("ps", c)])
                k.op("pe", lambda e: e.matmul(PS[c][:, 0:w_], lhsT=qr[:, 1, qsl], rhs=kr[:, 1, ksl], start=False, stop=True), r=rk_, w=[("ps", c)])
                k.op("dve", lambda e: e.reduce_max(out=m4[:, c:c + 1], in_=PS[c][:, 0:w_], axis=AX.X), r=[("ps", c)], w=[("m4", c)])
            k.op("dve", lambda e: e.reduce_max(out=mx[:], in_=m4[:, 0:nch], axis=AX.X), r=[("m4", c) for c in range(nch)], w=["mx"])
            k.op("dve", lambda e: e.tensor_scalar_mul(out=mx[:], in0=mx[:], scalar1=-1.0), r=["mx"], w=["mx"])
            lc = nch - 1
            wl = nk - lc * 512
            k.op("dve", lambda e: e.tensor_tensor(out=dtile[:], in0=PS[lc][:, wl - 128:wl], in1=cm[:], op=ALU.add), r=[("ps", lc), "cm"], w=["dtile"])
            k.op("dve", lambda e: e.memset(rsum[:], 0.0), w=["rsum"])
            for c in range(nch):
                w_ = min(512, nk - c * 512)
                if c == lc:
                    w_ -= 128
                if w_ > 0:
                    k.op("act", lambda e: e.activation(out=P[:, c * 512:c * 512 + w_], in_=PS[c][:, 0:w_], func=AF.Exp, bias=mx[:, 0:1], scale=1.0,
                                                       accum_out=rsum[:, c:c + 1]), r=[("ps", c), "mx", "rsum"], w=[KP, "rsum"])
            k.op("act", lambda e: e.activation(out=P[:, nk - 128:nk], in_=dtile[:], func=AF.Exp, bias=mx[:, 0:1], scale=1.0, accum_out=rsum[:, 4:5]),
                 r=["dtile", "mx", "rsum"], w=[KP, "rsum"])
            k.op("dve", lambda e: e.reduce_sum(out=rinv[:], in_=rsum[:], axis=AX.X), r=["rsum"], w=["rinv"])
            k.op("dve", lambda e: e.reciprocal(out=rinv[:], in_=rinv[:]), r=["rinv"], w=["rinv"])
            k.op("dve", lambda e: e.tensor_scalar_mul(out=dg[:], in0=ident[:], scalar1=rinv[:, 0:1]), r=["ident", "rinv"], w=["dg"])
            nkt = nk // 128
            for g0 in range(0, nkt, 4):
                gi = (g0 // 4) % 2
                PTs = arena(o_PT + gi * 256, 512, BF16).rearrange("p (j q) -> p j q", j=4)
                n_ = min(4, nkt - g0)
                for j in range(n_):
                    kt = g0 + j
                    k.op("pe", lambda e: e.matmul(PS[5 + gi][:, j * 128:(j + 1) * 128], lhsT=P[:, kt * 128:(kt + 1) * 128], rhs=dg[:], start=True, stop=True),
                         r=[KP, "dg"], w=[("ps", 5 + gi)])
                k.op("act", lambda e: e.copy(out=PTs[:, 0:n_, :], in_=PS[5 + gi][:, 0:n_ * 128].rearrange("p (j q) -> p j q", j=n_)), r=[("ps", 5 + gi)], w=[("PT", gi)])
                for j in range(n_):
                    kt = g0 + j
                    k.op("pe", lambda e: e.matmul(PS[7][:, 0:128], lhsT=V[:, kt, :], rhs=PTs[:, j, :], start=(kt == 0), stop=(kt == nkt - 1)),
                         r=[KH + ("V", kt // 4), ("PT", gi)], w=[("ps", 7)])
            k.op("act", lambda e: e.copy(out=osb[:, qsl], in_=PS[7][:, 0:128]), r=[("ps", 7)], w=[("osb", hb)])
        outs.append(k.dma("sp", oT[h * 128:(h + 1) * 128, 0:nqt * 128], osb[:, 0:nqt * 128], r=[("osb", hb)]))
    k.finish(outs)
    k.close()
    return nc


GDN_S = 2048
GDN_D = 2048
GDN_RMS_EPS = 1e-6
GDN_QSCALE = 128 ** -0.5
GDN_NG = 8


def build_A_gdn(ngroups=GDN_NG, ntiles=16):
    k = KB()
    nc = k.nc
    xT = k.dram("xT", [GDN_D, GDN_S])
    pp = k.dram("pp", [128, 2, 16])
    wq = k.dram("wq", [GDN_D, 1024])
    wk = k.dram("wk", [GDN_D, 1024])
    wv = k.dram("wv", [GDN_D, 2048])
    wz = k.dram("wz", [GDN_D, 2048])
    wba = k.dram("wba", [GDN_D, 32])
    cw = k.dram("cw", [128, 32, 4])
    hv = k.dram("hv", [128, 2, 16])
    ngp = k.dram("ngp", [128, 1])
    cst = k.dram("cst", [128, 7, 128])
    oT = k.dram("oT", [16 * 128, GDN_S], BF16, kind="ExternalOutput")

    uT = k.sb("uT", [128, 16, GDN_S], BF16)
    xb = [k.sb(f"xb{i}", [128, GDN_S]) for i in range(2)]
    xpre = k.sb("xpre", [128, GDN_S + 3])
    cacc = k.sb("cacc", [128, GDN_S])
    G = k.sb("G", [128, 6, GDN_S], BF16)
    Wb = [k.sb(f"Wb{i}", [128, 16, 128], BF16) for i in range(2)]
    wbas = k.sb("wbas", [128, 16, 32], BF16)
    cws = k.sb("cws", [128, 32, 4])
    hvs = k.sb("hvs", [128, 2, 16])
    ngs = k.sb("ngs", [128, 1])
    cs = k.sb("cs", [128, 7, 128])
    pps = k.sb("pps", [128, 2, 16])
    ones = k.sb("ones", [128, 128])
    identb = k.sb("identb", [128, 128], BF16)
    beta = k.sb("beta", [128, 16, 16])
    gt = k.sb("gt", [128, 16, 16])
    gc = k.sb("gc", [128, 16, 16])
    egc = k.sb("egc", [128, 16, 16])
    bge = k.sb("bge", [128, 16, 16])
    ekt = k.sb("ekt", [128, 16, 16])
    egl = k.sb("egl", [128, 2, 16, 16])
    tA = k.sb("tA", [128, 16, 16])
    tB = k.sb("tB", [128, 16, 16])
    vb = k.sb("vb", [128, 2, 128], BF16)
    kbg = k.sb("kbg", [128, 2, 128], BF16)
    ktl = k.sb("ktl", [128, 2, 128], BF16)
    Rg = k.sb("Rg", [128, 2, 64])
    Rb = k.sb("Rb", [128, 2, 64])
    Dm = k.sb("Dm", [128, 2, 64])
    dec = k.sb("dec", [128, 2, 64])
    bs = k.sb("bs", [128, 2, 64])
    tmpm = k.sb("tmpm", [128, 2, 64])
    aT = k.sb("aT", [128, 2, 64], BF16)
    Sm = [k.sb(f"Sm{i}", [128, 2, 64], BF16) for i in range(2)]
    STm = [k.sb(f"STm{i}", [128, 2, 64], BF16) for i in range(2)]
    Xm = [k.sb(f"Xm{i}", [128, 2, 64], BF16) for i in range(2)]
    XTm = [k.sb(f"XTm{i}", [128, 2, 64], BF16) for i in range(2)]
    uval = k.sb("uval", [128, 2, 128])
    wdT = k.sb("wdT", [128, 2, 128], BF16)
    vnew = k.sb("vnew", [128, 2, 128], BF16)
    o1 = k.sb("o1", [128, 2, 128])
    otok = k.sb("otok", [128, 2, 128])
    on = k.sb("on", [128, 2, 128], BF16)
    ss = k.sb("ss", [128, 2])
    junk = k.sb("junk", [128, 128])
    state = k.sb("state", [128, 2, 128])
    state_bf = k.sb("state_bf", [128, 2, 128], BF16)
    osb = k.sb("osb", [128, 2, GDN_S], BF16)
    rstd = k.sb("rstd", [128, 512])
    sq = k.sb("sq", [128, 512])
    PS = [k.ps(f"ps{i}", [128, 512]) for i in range(8)]

    k.dma("sp", pps[:], pp[:, :, :], w=["pps"])
    k.dma("sp", cws[:], cw[:, :, :], w=["cws"])
    k.dma("sp", hvs[:], hv[:, :, :], w=["hvs"])
    k.dma("sp", ngs[:], ngp[:, :], w=["ngs"])
    k.dma("sp", cs[:], cst[:, :, :], w=["cs"])
    k.dma("pool", wbas[:], wba.rearrange("(c p) n -> p c n", p=128), w=["wbas"])
    Ublk, Oblk, OnesC, identf = cs[:, 0, :], cs[:, 1, :], [cs[:, 2, :], cs[:, 3, :]], cs[:, 4, :]
    Irep, tri_i, tri_s = cs[:, 5, 0:64], cs[:, 5, 64:128], cs[:, 6, 0:64]
    k.op("dve", lambda e: e.tensor_scalar_add(out=pps[:, 0, :], in0=pps[:, 0, :], scalar1=1.0), r=["pps"], w=["pps"])
    k.op("pool", lambda e: e.memset(ones[:], 1.0), w=["ones"])
    k.op("pool", lambda e: e.memset(xpre[:, 0:3], 0.0), w=["xpre0"])
    k.op("dve", lambda e: e.tensor_copy(out=identb[:], in_=identf), r=["cs"], w=["identb"])
    for c in range(16):
        k.dma("sp", xb[c % 2][:], xT[c * 128:(c + 1) * 128, :], w=[("xb", c % 2)])
        k.op("act", lambda e: e.activation(out=uT[:, c, :], in_=xb[c % 2][:], func=AF.Identity, scale=pps[:, 0, c:c + 1], bias=pps[:, 1, c:c + 1]),
             r=[("xb", c % 2), "pps"], w=[("uT", c)])
    RU = [("uT", c) for c in range(16)]

    for tt in range(16):
        for c in range(16):
            k.op("pe", lambda e: e.matmul(PS[0][:, tt * 32:(tt + 1) * 32], lhsT=uT[:, c, tt * 128:(tt + 1) * 128], rhs=wbas[:, c, :], start=(c == 0), stop=(c == 15)),
                 r=RU + ["wbas"], w=[("ps", 0)])
    ba = PS[0][:].rearrange("p (t n) -> p t n", t=16)
    k.op("act", lambda e: e.activation(out=beta[:], in_=ba[:, :, 0:16], func=AF.Sigmoid), r=[("ps", 0)], w=["beta"])
    for tt in range(16):
        k.op("dve", lambda e: e.tensor_tensor(out=tA[:, tt, :], in0=ba[:, tt, 16:32], in1=hvs[:, 1, :], op=ALU.add), r=[("ps", 0), "hvs"], w=["tA"])
    k.op("act", lambda e: e.activation(out=tB[:], in_=tA[:], func=AF.Abs), r=["tA"], w=["tB"])
    k.op("act", lambda e: e.activation(out=tB[:], in_=tB[:], func=AF.Exp, scale=-1.0), r=["tB"], w=["tB"])
    k.op("dve", lambda e: e.tensor_scalar_add(out=tB[:], in0=tB[:], scalar1=1.0), r=["tB"], w=["tB"])
    k.op("act", lambda e: e.activation(out=tB[:], in_=tB[:], func=AF.Ln), r=["tB"], w=["tB"])
    k.op("dve", lambda e: e.tensor_scalar_max(out=tA[:], in0=tA[:], scalar1=0.0), r=["tA"], w=["tA"])
    k.op("dve", lambda e: e.tensor_tensor(out=tA[:], in0=tA[:], in1=tB[:], op=ALU.add), r=["tA", "tB"], w=["tA"])
    k.op("act", lambda e: e.activation(out=hvs[:, 0, :], in_=hvs[:, 0, :], func=AF.Exp), r=["hvs"], w=["hvs"])
    for tt in range(16):
        k.op("dve", lambda e: e.scalar_tensor_tensor(out=gt[:, tt, :], in0=tA[:, tt, :], scalar=-1.0, in1=hvs[:, 0, :], op0=ALU.mult, op1=ALU.mult),
             r=["tA", "hvs"], w=["gt"])
    gflat = gt[:].rearrange("p t h -> p (t h)")
    k.op("pe", lambda e: e.matmul(PS[1][:, 0:256], lhsT=Ublk, rhs=gflat, start=True, stop=True), r=["gt", "cs"], w=[("ps", 1)])
    k.op("pe", lambda e: e.matmul(PS[1][:, 256:512], lhsT=Oblk, rhs=gflat, start=True, stop=True), r=["gt", "cs"], w=[("ps", 1)])
    for c2 in range(2):
        k.op("pe", lambda e: e.matmul(PS[2][:, c2 * 256:(c2 + 1) * 256], lhsT=OnesC[c2], rhs=gflat, start=True, stop=True), r=["gt", "cs"], w=[("ps", 2)])
    k.op("act", lambda e: e.copy(out=gc[:].rearrange("p t h -> p (t h)"), in_=PS[1][:, 0:256]), r=[("ps", 1)], w=["gc"])
    k.op("act", lambda e: e.activation(out=egc[:].rearrange("p t h -> p (t h)"), in_=PS[1][:, 0:256], func=AF.Exp), r=[("ps", 1)], w=["egc"])
    k.op("dve", lambda e: e.tensor_tensor(out=ekt[:].rearrange("p t h -> p (t h)"), in0=PS[1][:, 256:512], in1=gc[:].rearrange("p t h -> p (t h)"), op=ALU.subtract),
         r=[("ps", 1), "gc"], w=["ekt"])
    k.op("act", lambda e: e.activation(out=ekt[:], in_=ekt[:], func=AF.Exp), r=["ekt"], w=["ekt"])
    k.op("act", lambda e: e.activation(out=egl[:].rearrange("p c t h -> p (c t h)"), in_=PS[2][:], func=AF.Exp), r=[("ps", 2)], w=["egl"])
    k.op("dve", lambda e: e.tensor_tensor(out=bge[:], in0=beta[:], in1=egc[:], op=ALU.mult), r=["beta", "egc"], w=["bge"])
    GATES = ["beta", "gc", "egc", "bge", "ekt", "egl"]

    wcount = [0]

    def proj_block(wsrc, col0, evac):
        i = wcount[0]; wcount[0] += 1
        wb = Wb[i % 2]
        k.dma("pool", wb[:], wsrc[:, col0:col0 + 128].rearrange("(c p) n -> p c n", p=128), w=[("Wb", i % 2)])
        for tc_ in range(4):
            pi = tc_ % 4
            for c in range(16):
                k.op("pe", lambda e: e.matmul(PS[pi][:], lhsT=wb[:, c, :], rhs=uT[:, c, tc_ * 512:(tc_ + 1) * 512], start=(c == 0), stop=(c == 15)),
                     r=RU + [("Wb", i % 2)], w=[("ps", pi)])
            evac(tc_, PS[pi][:], ("ps", pi))

    def conv_silu(blk, dst, l2, scale):
        k.op("dve", lambda e: e.tensor_scalar(out=cacc[:], in0=xpre[:, 3:GDN_S + 3], scalar1=cws[:, blk, 3:4], scalar2=None, op0=ALU.mult), r=["xpre", "xpre0", "cws"], w=["cacc"])
        for j in range(3):
            k.op("dve", lambda e: e.scalar_tensor_tensor(out=cacc[:], in0=xpre[:, j:GDN_S + j], scalar=cws[:, blk, j:j + 1], in1=cacc[:], op0=ALU.mult, op1=ALU.add),
                 r=["xpre", "xpre0", "cws", "cacc"], w=["cacc"])
        if not l2:
            k.op("act", lambda e: e.activation(out=dst, in_=cacc[:], func=AF.Silu), r=["cacc"], w=["G"])
            return
        k.op("act", lambda e: e.activation(out=cacc[:], in_=cacc[:], func=AF.Silu), r=["cacc"], w=["cacc"])
        for tc_ in range(4):
            tsl = slice(tc_ * 512, (tc_ + 1) * 512)
            k.op("act", lambda e: e.activation(out=sq[:], in_=cacc[:, tsl], func=AF.Square), r=["cacc"], w=["sq"])
            k.op("pe", lambda e: e.matmul(PS[4][:], lhsT=ones[:], rhs=sq[:], start=True, stop=True), r=["sq", "ones"], w=[("ps", 4)])
            k.op("dve", lambda e: e.tensor_scalar_add(out=rstd[:], in0=PS[4][:], scalar1=1e-6), r=[("ps", 4)], w=["rstd"])
            k.op("act", lambda e: e.activation(out=rstd[:], in_=rstd[:], func=AF.Sqrt), r=["rstd"], w=["rstd"])
            k.op("dve", lambda e: e.reciprocal(out=rstd[:], in_=rstd[:]), r=["rstd"], w=["rstd"])
            k.op("dve", lambda e: e.scalar_tensor_tensor(out=dst[:, tsl], in0=cacc[:, tsl], scalar=scale, in1=rstd[:], op0=ALU.mult, op1=ALU.mult), r=["cacc", "rstd"], w=["G"])

    outs = []
    for g in range(ngroups):
        def ev_pre(tc_, ps, pk):
            k.op("act", lambda e: e.copy(out=xpre[:, 3 + tc_ * 512:3 + (tc_ + 1) * 512], in_=ps), r=[pk], w=["xpre"])
        proj_block(wq, g * 128, ev_pre)
        conv_silu(g, G[:, 0, :], True, GDN_QSCALE)
        proj_block(wk, g * 128, ev_pre)
        conv_silu(8 + g, G[:, 1, :], True, 1.0)
        for a in range(2):
            proj_block(wv, (2 * g + a) * 128, ev_pre)
            conv_silu(16 + 2 * g + a, G[:, 2 + a, :], False, 1.0)
        for a in range(2):
            def ev_z(tc_, ps, pk, a=a):
                k.op("act", lambda e: e.activation(out=G[:, 4 + a, tc_ * 512:(tc_ + 1) * 512], in_=ps, func=AF.Silu), r=[pk], w=["G"])
            proj_block(wz, (2 * g + a) * 128, ev_z)
        qT, kT = G[:, 0, :], G[:, 1, :]
        k.op("dve", lambda e: e.memset(state[:], 0.0), w=["state"])
        k.op("dve", lambda e: e.memset(state_bf[:], 0.0), w=["state_bf"])
        for tt in range(ntiles):
            tsl = slice(tt * 128, (tt + 1) * 128)
            H = [2 * g, 2 * g + 1]
            k.op("pe", lambda e: e.matmul(PS[0][:, 0:128], lhsT=kT[:, tsl], rhs=identb[:], start=True, stop=True), r=["G", "identb"], w=[("ps", 0)])
            for a in range(2):
                k.op("pe", lambda e: e.matmul(PS[0][:, 128 + a * 128:256 + a * 128], lhsT=G[:, 2 + a, tsl], rhs=identb[:], start=True, stop=True), r=["G", "identb"], w=[("ps", 0)])
            for a in range(2):
                h = H[a]
                k.op("dve", lambda e: e.tensor_scalar(out=vb[:, a, :], in0=PS[0][:, 128 + a * 128:256 + a * 128], scalar1=beta[:, tt, h:h + 1], scalar2=None, op0=ALU.mult),
                     r=[("ps", 0)] + GATES, w=["vb"])
                k.op("dve", lambda e: e.tensor_scalar(out=kbg[:, a, :], in0=PS[0][:, 0:128], scalar1=bge[:, tt, h:h + 1], scalar2=None, op0=ALU.mult), r=[("ps", 0)] + GATES, w=["kbg"])
                k.op("dve", lambda e: e.tensor_scalar(out=ktl[:, a, :], in0=PS[0][:, 0:128], scalar1=ekt[:, tt, h:h + 1], scalar2=None, op0=ALU.mult), r=[("ps", 0)] + GATES, w=["ktl"])
                k.op("dve", lambda e: e.tensor_scalar(out=Rg[:, a, :], in0=Irep, scalar1=gc[:, tt, h:h + 1], scalar2=None, op0=ALU.mult), r=["cs"] + GATES, w=["Rg"])
                k.op("dve", lambda e: e.tensor_scalar(out=Rb[:, a, :], in0=Irep, scalar1=beta[:, tt, h:h + 1], scalar2=None, op0=ALU.mult), r=["cs"] + GATES, w=["Rb"])
            for c2 in range(2):
                cs_ = slice(c2 * 64, (c2 + 1) * 64)
                tcs = slice(tt * 128 + c2 * 64, tt * 128 + (c2 + 1) * 64)
                k.op("pe", lambda e: e.matmul(PS[1][cs_, 0:64], lhsT=kT[:, tcs], rhs=kT[:, tcs], start=True, stop=True), r=["G"], w=[("ps", 1)])
                k.op("pe", lambda e: e.matmul(PS[1][cs_, 64:128], lhsT=kT[:, tcs], rhs=qT[:, tcs], start=True, stop=True), r=["G"], w=[("ps", 1)])
            k.op("pe", lambda e: e.matmul(PS[2][:, 0:128], lhsT=Oblk, rhs=Rg[:].rearrange("p a i -> p (a i)"), start=True, stop=True), r=["Rg", "cs"], w=[("ps", 2)])
            k.op("pe", lambda e: e.matmul(PS[2][:, 128:256], lhsT=Oblk, rhs=Rb[:].rearrange("p a i -> p (a i)"), start=True, stop=True), r=["Rb", "cs"], w=[("ps", 2)])
            for a in range(2):
                h = H[a]
                k.op("dve", lambda e: e.tensor_scalar(out=Dm[:, a, :], in0=PS[2][:, a * 64:(a + 1) * 64], scalar1=gc[:, tt, h:h + 1], scalar2=0.0, op0=ALU.subtract, op1=ALU.min),
                     r=[("ps", 2)] + GATES, w=["Dm"])
                k.op("dve", lambda e: e.tensor_tensor(out=bs[:, a, :], in0=PS[2][:, 128 + a * 64:128 + (a + 1) * 64], in1=tri_s, op=ALU.mult), r=[("ps", 2), "cs"], w=["bs"])
            k.op("act", lambda e: e.activation(out=dec[:], in_=Dm[:], func=AF.Exp), r=["Dm"], w=["dec"])
            for a in range(2):
                k.op("dve", lambda e: e.tensor_tensor(out=dec[:, a, :], in0=dec[:, a, :], in1=tri_i, op=ALU.mult), r=["dec", "cs"], w=["dec"])
            for a in range(2):
                k.op("dve", lambda e: e.tensor_tensor(out=aT[:, a, :], in0=PS[1][:, 64:128], in1=dec[:, a, :], op=ALU.mult), r=[("ps", 1), "dec"], w=["aT"])
                k.op("dve", lambda e: e.tensor_tensor(out=tmpm[:, a, :], in0=PS[1][:, 0:64], in1=dec[:, a, :], op=ALU.mult), r=[("ps", 1), "dec"], w=["tmpm"])
            k.op("dve", lambda e: e.scalar_tensor_tensor(out=Sm[0][:], in0=tmpm[:], scalar=-1.0, in1=bs[:], op0=ALU.mult, op1=ALU.mult), r=["tmpm", "bs"], w=[("Sm", 0)])
            for c2 in range(2):
                cs_ = slice(c2 * 64, (c2 + 1) * 64)
                for a in range(2):
                    k.op("pe", lambda e: e.matmul(PS[3][cs_, 128 + a * 64:128 + (a + 1) * 64], lhsT=Sm[0][cs_, a, :], rhs=identb[cs_, cs_], start=True, stop=True),
                         r=[("Sm", 0), "identb"], w=[("ps", 3)])
            k.op("act", lambda e: e.copy(out=STm[0][:].rearrange("p a i -> p (a i)"), in_=PS[3][:, 128:256]), r=[("ps", 3)], w=[("STm", 0)])
            for a in range(2):
                k.op("dve", lambda e: e.tensor_tensor(out=Xm[0][:, a, :], in0=Sm[0][:, a, :], in1=Irep, op=ALU.add), r=[("Sm", 0), "cs"], w=[("Xm", 0)])
                k.op("dve", lambda e: e.tensor_tensor(out=XTm[0][:, a, :], in0=STm[0][:, a, :], in1=Irep, op=ALU.add), r=[("STm", 0), "cs"], w=[("XTm", 0)])

            def mmset(bank, col0, L, R_, rk):
                for c2 in range(2):
                    cs_ = slice(c2 * 64, (c2 + 1) * 64)
                    for a in range(2):
                        k.op("pe", lambda e: e.matmul(PS[bank][cs_, col0 + a * 64:col0 + (a + 1) * 64], lhsT=L[cs_, a, :], rhs=R_[cs_, a, :], start=True, stop=True),
                             r=rk, w=[("ps", bank)])
            cur = 0
            for lvl in range(1, 6):
                nx = 1 - cur
                last = (lvl == 5)
                mmset(3, 0, STm[cur], Sm[cur], [("Sm", cur), ("STm", cur)])
                if not last:
                    mmset(3, 128, Sm[cur], STm[cur], [("Sm", cur), ("STm", cur)])
                k.op("act", lambda e: e.copy(out=Sm[nx][:].rearrange("p a i -> p (a i)"), in_=PS[3][:, 0:128]), r=[("ps", 3)], w=[("Sm", nx)])
                if not last:
                    k.op("act", lambda e: e.copy(out=STm[nx][:].rearrange("p a i -> p (a i)"), in_=PS[3][:, 128:256]), r=[("ps", 3)], w=[("STm", nx)])
                mmset(4, 0, XTm[cur], Sm[nx], [("XTm", cur), ("Sm", nx)])
                if not last:
                    mmset(4, 128, Sm[nx], XTm[cur], [("XTm", cur), ("Sm", nx)])
                k.op("dve", lambda e: e.tensor_tensor(out=Xm[nx][:].rearrange("p a i -> p (a i)"), in0=PS[4][:, 0:128], in1=Xm[cur][:].rearrange("p a i -> p (a i)"), op=ALU.add),
                     r=[("ps", 4), ("Xm", cur)], w=[("Xm", nx)])
                if not last:
                    k.op("dve", lambda e: e.tensor_tensor(out=XTm[nx][:].rearrange("p a i -> p (a i)"), in0=PS[4][:, 128:256], in1=XTm[cur][:].rearrange("p a i -> p (a i)"), op=ALU.add),
                         r=[("ps", 4), ("XTm", cur)], w=[("XTm", nx)])
                cur = nx
            X = Xm[cur]
            KX = ("Xm", cur)
            for c2 in range(2):
                cs_ = slice(c2 * 64, (c2 + 1) * 64)
                for a in range(2):
                    k.op("pe", lambda e: e.matmul(PS[5][cs_, a * 128:(a + 1) * 128], lhsT=X[cs_, a, :], rhs=vb[cs_, a, :], start=True, stop=True), r=[KX, "vb"], w=[("ps", 5)])
                    k.op("pe", lambda e: e.matmul(PS[5][:, 256 + a * 128 + c2 * 64:256 + a * 128 + (c2 + 1) * 64], lhsT=kbg[cs_, a, :], rhs=X[cs_, a, :], start=True, stop=True),
                         r=[KX, "kbg"], w=[("ps", 5)])
            k.op("act", lambda e: e.copy(out=uval[:].rearrange("p a e -> p (a e)"), in_=PS[5][:, 0:256]), r=[("ps", 5)], w=["uval"])
            k.op("act", lambda e: e.copy(out=wdT[:].rearrange("p a e -> p (a e)"), in_=PS[5][:, 256:512]), r=[("ps", 5)], w=["wdT"])
            for c2 in range(2):
                cs_ = slice(c2 * 64, (c2 + 1) * 64)
                tcs = slice(tt * 128 + c2 * 64, tt * 128 + (c2 + 1) * 64)
                for a in range(2):
                    h = H[a]
                    k.op("pe", lambda e: e.matmul(PS[6][cs_, a * 128:(a + 1) * 128], lhsT=wdT[:, a, cs_], rhs=state_bf[:, a, :], start=True, stop=True), r=["wdT", "state_bf"], w=[("ps", 6)])
                    k.op("pe", lambda e: e.matmul(PS[6][cs_, 256 + a * 128:256 + (a + 1) * 128], lhsT=qT[:, tcs], rhs=state_bf[:, a, :], start=True, stop=True), r=["G", "state_bf"], w=[("ps", 6)])
                for a in range(2):
                    h = H[a]
                    k.op("dve", lambda e: e.tensor_tensor(out=vnew[cs_, a, :], in0=uval[cs_, a, :], in1=PS[6][cs_, a * 128:(a + 1) * 128], op=ALU.subtract), r=["uval", ("ps", 6)], w=["vnew"])
                    k.op("act", lambda e: e.activation(out=o1[cs_, a, :], in_=PS[6][cs_, 256 + a * 128:256 + (a + 1) * 128], func=AF.Copy, scale=egc[cs_, tt, h:h + 1]),
                         r=[("ps", 6)] + GATES, w=["o1"])
                for a in range(2):
                    k.op("pe", lambda e: e.matmul(PS[7][cs_, a * 128:(a + 1) * 128], lhsT=aT[cs_, a, :], rhs=vnew[cs_, a, :], start=True, stop=True), r=["aT", "vnew"], w=[("ps", 7)])
                    k.op("pe", lambda e: e.matmul(PS[7][:, 256 + a * 128:256 + (a + 1) * 128], lhsT=ktl[cs_, a, :], rhs=vnew[cs_, a, :], start=True, stop=True), r=["ktl", "vnew"], w=[("ps", 7)])
                for a in range(2):
                    h = H[a]
                    k.op("dve", lambda e: e.tensor_tensor(out=otok[cs_, a, :], in0=PS[7][cs_, a * 128:(a + 1) * 128], in1=o1[cs_, a, :], op=ALU.add), r=[("ps", 7), "o1"], w=["otok"])
                    k.op("dve", lambda e: e.scalar_tensor_tensor(out=state[:, a, :], in0=state[:, a, :], scalar=egl[:, c2, tt, h:h + 1], in1=PS[7][:, 256 + a * 128:256 + (a + 1) * 128],
                                                                 op0=ALU.mult, op1=ALU.add), r=["state", ("ps", 7)] + GATES, w=["state"])
                    k.op("act", lambda e: e.copy(out=state_bf[:, a, :], in_=state[:, a, :]), r=["state"], w=["state_bf"])
            for a in range(2):
                k.op("act", lambda e: e.activation(out=junk[:], in_=otok[:, a, :], func=AF.Square, accum_out=ss[:, a:a + 1]), r=["otok"], w=["junk", "ss"])
            k.op("dve", lambda e: e.tensor_scalar(out=ss[:], in0=ss[:], scalar1=1.0 / 128, scalar2=GDN_RMS_EPS, op0=ALU.mult, op1=ALU.add), r=["ss"], w=["ss"])
            k.op("act", lambda e: e.activation(out=ss[:], in_=ss[:], func=AF.Sqrt), r=["ss"], w=["ss"])
            k.op("dve", lambda e: e.reciprocal(out=ss[:], in_=ss[:]), r=["ss"], w=["ss"])
            for a in range(2):
                k.op("dve", lambda e: e.tensor_scalar(out=on[:, a, :], in0=otok[:, a, :], scalar1=ss[:, a:a + 1], scalar2=None, op0=ALU.mult), r=["otok", "ss"], w=["on"])
                k.op("pe", lambda e: e.matmul(PS[1][:, 128 + a * 128:256 + a * 128], lhsT=on[:, a, :], rhs=identb[:], start=True, stop=True), r=["on", "identb"], w=[("ps", 1)])
            for a in range(2):
                k.op("dve", lambda e: e.scalar_tensor_tensor(out=osb[:, a, tsl], in0=PS[1][:, 128 + a * 128:256 + a * 128], scalar=ngs[:, 0:1], in1=G[:, 4 + a, tsl], op0=ALU.mult, op1=ALU.mult),
                     r=[("ps", 1), "ngs", "G"], w=[("osb", a)])
        for a in range(2):
            outs.append(k.dma("sp", oT[(2 * g + a) * 128:(2 * g + a + 1) * 128, 0:ntiles * 128], osb[:, a, 0:ntiles * 128], r=[("osb", a)]))
    k.finish(outs)
    k.close()
    return nc


GLA_S = 2048
GLA_D = 2048
GLA_RMS_EPS = 1e-6
GLA_QSCALE = 256 ** -0.5


def build_A_gla(nheads=2, ntiles=16):
    k = KB()
    nc = k.nc
    xT = k.dram("xT", [GLA_D, GLA_S])
    pp = k.dram("pp", [128, 2, 16])
    w_q = k.dram("w_q", [GLA_D, 512])
    w_k = k.dram("w_k", [GLA_D, 512])
    w_v = k.dram("w_v", [GLA_D, 1024])
    w_g = k.dram("w_g", [GLA_D, 1024])
    w_gl = k.dram("w_gl", [GLA_D, 16])
    w_gk = k.dram("w_gk", [17, 512])
    ngb = k.dram("ngb", [128, 512])
    cst = k.dram("cst", [128, 3, 128])
    o_tok = k.dram("o_tok", [GLA_S, 1024], BF16, kind="ExternalOutput")

    uT = k.sb("uT", [128, 16, GLA_S], BF16)
    W = k.sb("W", [128, 16, 1552], BF16)
    wgk = k.sb("wgk", [17, 512])
    ngs = k.sb("ngs", [128, 512])
    cs = k.sb("cs", [128, 3, 128])
    pps = k.sb("pps", [128, 2, 16])
    xb = [k.sb(f"xb{i}", [128, GLA_S]) for i in range(2)]
    glT = k.sb("glT", [17, 128])
    state = k.sb("state", [128, 2, 512])
    state_bf = k.sb("state_bf", [128, 2, 512], BF16)
    k_tok = k.sb("k_tok", [128, 256])
    v_tok = k.sb("v_tok", [128, 512], BF16)
    sg = k.sb("sg", [128, 512])
    qT = k.sb("qT", [128, 2, 128])
    kT = k.sb("kT", [128, 2, 128])
    la = k.sb("la", [128, 256])
    t1 = k.sb("t1", [128, 256])
    t2 = k.sb("t2", [128, 256])
    bT = k.sb("bT", [128, 2, 128])
    ebT = k.sb("ebT", [128, 2, 128])
    enbT = k.sb("enbT", [128, 2, 128])
    ebl = k.sb("ebl", [128, 2, 2])
    qd = k.sb("qd", [128, 2, 128], BF16)
    ki = k.sb("ki", [128, 2, 128], BF16)
    ktl = k.sb("ktl", [128, 256], BF16)
    aT = k.sb("aT", [128, 64], BF16)
    ss = k.sb("ss", [128, 1])
    junk = k.sb("junk", [128, 512])
    ot = [k.sb(f"ot{i}", [128, 512], BF16) for i in range(2)]
    PS = [k.ps(f"ps{i}", [128, 512]) for i in range(8)]

    k.dma("sp", pps[:], pp[:, :, :], w=["pps"])
    k.dma("sp", wgk[:], w_gk[:, :], w=["wgk"])
    k.dma("sp", ngs[:], ngb[:, :], w=["ngs"])
    k.dma("sp", cs[:], cst[:, :, :], w=["cs"])
    k.op("dve", lambda e: e.tensor_scalar_add(out=pps[:, 0, :], in0=pps[:, 0, :], scalar1=1.0), r=["pps"], w=["pps"])
    k.op("pool", lambda e: e.memset(glT[:], 1.0), w=["glT"])
    Ublk, Oblk, tri = cs[:, 0, :], cs[:, 1, :], cs[:, 2, 0:64]
    for c in range(16):
        k.dma("sp", xb[c % 2][:], xT[c * 128:(c + 1) * 128, :], w=[("xb", c % 2)])
        k.op("act", lambda e: e.activation(out=uT[:, c, :], in_=xb[c % 2][:], func=AF.Identity, scale=pps[:, 0, c:c + 1], bias=pps[:, 1, c:c + 1]),
             r=[("xb", c % 2), "pps"], w=[("uT", c)])
    RU = [("uT", c) for c in range(16)]
    outs = []
    n_out = 0
    for h in range(nheads):
        for c in range(16):
            rows = slice(c * 128, (c + 1) * 128)
            k.dma("pool", W[:, c, 0:256], w_q[rows, h * 256:(h + 1) * 256], w=[("W", c)])
            k.dma("pool", W[:, c, 256:512], w_k[rows, h * 256:(h + 1) * 256], w=[("W", c)])
            k.dma("pool", W[:, c, 512:1024], w_v[rows, h * 512:(h + 1) * 512], w=[("W", c)])
            k.dma("pool", W[:, c, 1024:1536], w_g[rows, h * 512:(h + 1) * 512], w=[("W", c)])
            k.dma("pool", W[:, c, 1536:1552], w_gl[rows, :], w=[("W", c)])
        RW = [("W", c) for c in range(16)]
        k.op("dve", lambda e: e.memset(state[:], 0.0), w=["state"])
        k.op("dve", lambda e: e.memset(state_bf[:], 0.0), w=["state_bf"])
        for tt in range(ntiles):
            tsl = slice(tt * 128, (tt + 1) * 128)
            for c in range(16):
                k.op("pe", lambda e: e.matmul(PS[0][:, 0:256], lhsT=uT[:, c, tsl], rhs=W[:, c, 256:512], start=(c == 0), stop=(c == 15)), r=RU + RW, w=[("ps", 0)])
            k.op("act", lambda e: e.copy(out=k_tok[:], in_=PS[0][:, 0:256]), r=[("ps", 0)], w=["k_tok"])
            for c in range(16):
                k.op("pe", lambda e: e.matmul(PS[1][:], lhsT=uT[:, c, tsl], rhs=W[:, c, 512:1024], start=(c == 0), stop=(c == 15)), r=RU + RW, w=[("ps", 1)])
            k.op("act", lambda e: e.copy(out=v_tok[:], in_=PS[1][:]), r=[("ps", 1)], w=["v_tok"])
            for c in range(16):
                k.op("pe", lambda e: e.matmul(PS[2][:], lhsT=uT[:, c, tsl], rhs=W[:, c, 1024:1536], start=(c == 0), stop=(c == 15)), r=RU + RW, w=[("ps", 2)])
            k.op("act", lambda e: e.activation(out=sg[:], in_=PS[2][:], func=AF.Silu), r=[("ps", 2)], w=["sg"])
            for hf in range(2):
                for c in range(16):
                    k.op("pe", lambda e: e.matmul(PS[3][:, hf * 128:(hf + 1) * 128], lhsT=W[:, c, hf * 128:(hf + 1) * 128], rhs=uT[:, c, tsl], start=(c == 0), stop=(c == 15)),
                         r=RU + RW, w=[("ps", 3)])
                for c in range(16):
                    k.op("pe", lambda e: e.matmul(PS[3][:, 256 + hf * 128:256 + (hf + 1) * 128], lhsT=W[:, c, 256 + hf * 128:256 + (hf + 1) * 128], rhs=uT[:, c, tsl],
                                                  start=(c == 0), stop=(c == 15)), r=RU + RW, w=[("ps", 3)])
            k.op("act", lambda e: e.activation(out=qT[:].rearrange("p a t -> p (a t)"), in_=PS[3][:, 0:256], func=AF.Copy, scale=GLA_QSCALE), r=[("ps", 3)], w=["qT"])
            k.op("act", lambda e: e.copy(out=kT[:].rearrange("p a t -> p (a t)"), in_=PS[3][:, 256:512]), r=[("ps", 3)], w=["kT"])
            for c in range(16):
                k.op("pe", lambda e: e.matmul(PS[4][0:16, 0:128], lhsT=W[:, c, 1536:1552], rhs=uT[:, c, tsl], start=(c == 0), stop=(c == 15)), r=RU + RW, w=[("ps", 4)])
            k.op("act", lambda e: e.copy(out=glT[0:16, :], in_=PS[4][0:16, 0:128]), r=[("ps", 4), "glT"], w=["glT"])
            k.op("pe", lambda e: e.matmul(PS[0][:, 256:512], lhsT=glT[:], rhs=wgk[:, h * 256:(h + 1) * 256], start=True, stop=True), r=["glT", "wgk"], w=[("ps", 0)])
            gk = PS[0][:, 256:512]
            k.op("act", lambda e: e.activation(out=t1[:], in_=gk, func=AF.Abs), r=[("ps", 0)], w=["t1"])
            k.op("act", lambda e: e.activation(out=t1[:], in_=t1[:], func=AF.Exp, scale=-1.0), r=["t1"], w=["t1"])
            k.op("dve", lambda e: e.tensor_scalar_add(out=t1[:], in0=t1[:], scalar1=1.0), r=["t1"], w=["t1"])
            k.op("act", lambda e: e.activation(out=t1[:], in_=t1[:], func=AF.Ln), r=["t1"], w=["t1"])
            k.op("dve", lambda e: e.tensor_scalar_min(out=t2[:], in0=gk, scalar1=0.0), r=[("ps", 0)], w=["t2"])
            k.op("dve", lambda e: e.tensor_tensor(out=la[:], in0=t2[:], in1=t1[:], op=ALU.subtract), r=["t1", "t2"], w=["la"])
            k.op("dve", lambda e: e.tensor_scalar_mul(out=la[:], in0=la[:], scalar1=1.0 / 16.0), r=["la"], w=["la"])
            k.op("pe", lambda e: e.matmul(PS[5][:, 0:256], lhsT=Ublk, rhs=la[:], start=True, stop=True), r=["la", "cs"], w=[("ps", 5)])
            k.op("pe", lambda e: e.matmul(PS[5][:, 256:512], lhsT=Oblk, rhs=la[:], start=True, stop=True), r=["la", "cs"], w=[("ps", 5)])
            for hf in range(2):
                k.op("pe", lambda e: e.matmul(PS[4][:, 128 + hf * 128:256 + hf * 128], lhsT=la[:, hf * 128:(hf + 1) * 128], rhs=Ublk, start=True, stop=True),
                     r=["la", "cs"], w=[("ps", 4)])
            k.op("act", lambda e: e.copy(out=bT[:].rearrange("p a t -> p (a t)"), in_=PS[4][:, 128:384]), r=[("ps", 4)], w=["bT"])
            k.op("act", lambda e: e.activation(out=ebT[:], in_=bT[:], func=AF.Exp), r=["bT"], w=["ebT"])
            k.op("act", lambda e: e.activation(out=enbT[:], in_=bT[:], func=AF.Exp, scale=-1.0), r=["bT"], w=["enbT"])
            for hf in range(2):
                for c2 in range(2):
                    k.op("dve", lambda e: e.tensor_copy(out=ebl[:, hf, c2:c2 + 1], in_=ebT[:, hf, c2 * 64 + 63:c2 * 64 + 64]), r=["ebT"], w=["ebl"])
            k.op("dve", lambda e: e.tensor_copy(out=t1[:], in_=PS[5][:, 0:256]), r=[("ps", 5)], w=["t1"])
            k.op("dve", lambda e: e.tensor_tensor(out=t1[:], in0=PS[5][:, 256:512], in1=t1[:], op=ALU.subtract), r=[("ps", 5), "t1"], w=["t1"])
            k.op("act", lambda e: e.activation(out=t1[:], in_=t1[:], func=AF.Exp), r=["t1"], w=["t1"])
            k.op("dve", lambda e: e.tensor_tensor(out=ktl[:], in0=k_tok[:], in1=t1[:], op=ALU.mult), r=["k_tok", "t1"], w=["ktl"])
            k.op("dve", lambda e: e.tensor_tensor(out=qd[:], in0=qT[:], in1=ebT[:], op=ALU.mult), r=["qT", "ebT"], w=["qd"])
            k.op("dve", lambda e: e.tensor_tensor(out=ki[:], in0=kT[:], in1=enbT[:], op=ALU.mult), r=["kT", "enbT"], w=["ki"])
            for c2 in range(2):
                cs_ = slice(c2 * 64, (c2 + 1) * 64)
                for hf in range(2):
                    k.op("pe", lambda e: e.matmul(PS[4][cs_, 384:448], lhsT=ki[:, hf, cs_], rhs=qd[:, hf, cs_], start=(hf == 0), stop=(hf == 1)), r=["ki", "qd"], w=[("ps", 4)])
                k.op("dve", lambda e: e.tensor_tensor(out=aT[cs_, :], in0=PS[4][cs_, 384:448], in1=tri[cs_, :], op=ALU.mult), r=[("ps", 4), "cs"], w=["aT"])
                k.op("pe", lambda e: e.matmul(PS[6][cs_, :], lhsT=aT[cs_, :], rhs=v_tok[cs_, :], start=True, stop=False), r=["aT", "v_tok"], w=[("ps", 6)])
                for hf in range(2):
                    k.op("pe", lambda e: e.matmul(PS[6][cs_, :], lhsT=qd[:, hf, cs_], rhs=state_bf[:, hf, :], start=False, stop=(hf == 1)), r=["qd", "state_bf"], w=[("ps", 6)])
                for hf in range(2):
                    k.op("pe", lambda e: e.matmul(PS[7][:], lhsT=ktl[cs_, hf * 128:(hf + 1) * 128], rhs=v_tok[cs_, :], start=True, stop=True), r=["ktl", "v_tok"], w=[("ps", 7)])
                    k.op("dve", lambda e: e.scalar_tensor_tensor(out=state[:, hf, :], in0=state[:, hf, :], scalar=ebl[:, hf, c2:c2 + 1], in1=PS[7][:], op0=ALU.mult, op1=ALU.add),
                         r=["state", "ebl", ("ps", 7)], w=["state"])
                    k.op("act", lambda e: e.copy(out=state_bf[:, hf, :], in_=state[:, hf, :]), r=["state"], w=["state_bf"])
            k.op("act", lambda e: e.activation(out=junk[:], in_=PS[6][:], func=AF.Square, accum_out=ss[:]), r=[("ps", 6)], w=["junk", "ss"])
            k.op("dve", lambda e: e.tensor_scalar(out=ss[:], in0=ss[:], scalar1=1.0 / 512, scalar2=GLA_RMS_EPS, op0=ALU.mult, op1=ALU.add), r=["ss"], w=["ss"])
            k.op("act", lambda e: e.activation(out=ss[:], in_=ss[:], func=AF.Sqrt), r=["ss"], w=["ss"])
            k.op("dve", lambda e: e.reciprocal(out=ss[:], in_=ss[:]), r=["ss"], w=["ss"])
            k.op("dve", lambda e: e.scalar_tensor_tensor(out=junk[:], in0=PS[6][:], scalar=ss[:, 0:1], in1=ngs[:], op0=ALU.mult, op1=ALU.mult), r=[("ps", 6), "ss", "ngs", "junk"], w=["junk"])
            ob = ot[n_out % 2]
            k.op("dve", lambda e: e.tensor_tensor(out=ob[:], in0=junk[:], in1=sg[:], op=ALU.mult), r=["junk", "sg"], w=[("ot", n_out % 2)])
            outs.append(k.dma("sp", o_tok[tsl, h * 512:(h + 1) * 512], ob[:], r=[("ot", n_out % 2)]))
            n_out += 1
    k.finish(outs)
    k.close()
    return nc


MOBA_S = 2048
MOBA_D = 2048
MOBA_NH = 8
MOBA_SCALE = 128 ** -0.5
MOBA_NEG = -30000.0


def build_A_moba(nh=MOBA_NH, nqt=16, stop=99, dbg=0):
    k = KB()
    nc = k.nc
    xT = k.dram("xT", [MOBA_D, MOBA_S])
    pp = k.dram("pp", [128, 2, 16])
    w_q = k.dram("w_q", [MOBA_D, MOBA_NH * 128])
    w_k = k.dram("w_k", [MOBA_D, MOBA_NH * 128])
    w_v = k.dram("w_v", [MOBA_D, MOBA_NH * 128])
    tb = k.dram("tb", [128, MOBA_NH, 2, 128])
    cf = k.dram("cf", [128, MOBA_NH])
    oT = k.dram("oT", [MOBA_NH * 128, MOBA_S], BF16, kind="ExternalOutput")

    AR = k.sb("arena", [128, 27136])
    uT = k.sb("uT", [128, 16, MOBA_S], BF16)
    tbs = k.sb("tbs", [128, MOBA_NH, 2, 128])
    cfs = k.sb("cfs", [128, MOBA_NH])
    pps = k.sb("pps", [128, 2, 16])
    ident = k.sb("ident", [128, 128])
    mx = k.sb("mx", [128, 1])
    rsum = k.sb("rsum", [128, 5])
    rinv = k.sb("rinv", [128, 1])
    dg = k.sb("dg", [128, 128], BF16)
    kmean = k.sb("kmean", [128, 8])
    gate = k.sb("gate", [128, 8])
    m8 = k.sb("m8", [128, 8])
    selb = k.sb("selb", [128, 8])
    PS = [k.ps(f"ps{i}", [128, 512]) for i in range(8)]

    def arena(off, n, dt=F32):
        if dt == F32:
            return AR[:, off:off + n]
        return AR[:, off:off + n // 2].bitcast(BF16)

    k.op("pool", lambda e: e.memset(ident[:], 1.0), w=["ident"])
    k.op("pool", lambda e: e.affine_select(out=ident[:], in_=ident[:], pattern=[[-1, 128]], compare_op=ALU.is_equal,
                                           fill=0.0, base=0, channel_multiplier=1), r=["ident"], w=["ident"])
    k.dma("sp", pps[:], pp[:, :, :], w=["pps"])
    k.dma("sp", tbs[:], tb[:, :, :, :], w=["tbs"])
    k.dma("sp", cfs[:], cf[:, :], w=["cfs"])
    k.op("dve", lambda e: e.tensor_scalar_add(out=pps[:, 0, :], in0=pps[:, 0, :], scalar1=1.0), r=["pps"], w=["pps"])
    for h in range(MOBA_NH):
        k.op("dve", lambda e: e.tensor_scalar(out=tbs[:, h, 1, :], in0=tbs[:, h, 1, :], scalar1=cfs[:, h:h + 1], scalar2=None, op0=ALU.subtract),
             r=["tbs", "cfs"], w=["tbs"])

    o_V = 0
    o_wv = 8192
    o_x = o_wv + 8192
    assert o_x + 4096 <= 27136
    Vall = arena(o_V, 16 * MOBA_NH * 128, BF16).rearrange("p (t d) -> p t d", t=16)
    wv = arena(o_wv, 16 * MOBA_NH * 128, BF16).rearrange("p (c n) -> p c n", c=16)
    for c in range(16):
        k.dma("pool", wv[:, c, :], w_v[c * 128:(c + 1) * 128, :], w=[("wv", c)])
    for c in range(16):
        xb = arena(o_x + (c % 2) * 2048, 2048)
        k.dma("sp", xb, xT[c * 128:(c + 1) * 128, :], w=[("xb", c % 2)])
        k.op("act", lambda e: e.activation(out=uT[:, c, :], in_=xb, func=AF.Identity, scale=pps[:, 0, c:c + 1], bias=pps[:, 1, c:c + 1]),
             r=[("xb", c % 2), "pps"], w=[("uT", c)])
    for tt in range(16):
        for hc in range(2):
            pi = (tt * 2 + hc) % 4
            for c in range(16):
                k.op("pe", lambda e: e.matmul(PS[pi][:], lhsT=uT[:, c, tt * 128:(tt + 1) * 128], rhs=wv[:, c, hc * 512:(hc + 1) * 512], start=(c == 0), stop=(c == 15)),
                     r=[("uT", c), ("wv", c)], w=[("ps", pi)])
            k.op("act", lambda e: e.copy(out=Vall[:, tt, hc * 512:(hc + 1) * 512], in_=PS[pi][:]), r=[("ps", pi)], w=[("V", tt)])
    if stop == 1:
        t = k.dma("sp", oT[0:128, 0:1024], Vall[:, 0, :], r=[("V", 0)])
        k.finish([t]); k.close(); return nc
    if dbg != 3:
        k.barrier()
    o_wqk = 8192
    o_hd = o_wqk + 4096
    o_L = o_hd + 2 * 4096
    o_P = o_L + 2048
    o_PT = o_P + 2048
    o_o = o_PT + 512
    assert o_o + 2048 <= 27136, o_o
    L = arena(o_L, 2048)
    outs = []
    for h in range(nh):
        hb = h % 2
        wqh = arena(o_wqk + hb * 2048, 2048, BF16).rearrange("p (c n) -> p c n", c=16)
        wkh = arena(o_wqk + hb * 2048 + 1024, 2048, BF16).rearrange("p (c n) -> p c n", c=16)
        k.dma("pool", wqh[:], w_q[:, h * 128:(h + 1) * 128].rearrange("(c p) n -> p c n", p=128), w=[("wqh", hb)])
        k.dma("pool", wkh[:], w_k[:, h * 128:(h + 1) * 128].rearrange("(c p) n -> p c n", p=128), w=[("wkh", hb)])
        base = o_hd + hb * 4096
        qn = arena(base, 2048, BF16)
        kn = arena(base + 1024, 2048, BF16)
        q32 = arena(base + 2048, 2048)
        osb = arena(o_o + hb * 1024, 2048, BF16)
        KH = ("hd", hb)
        RU = [("uT", c) for c in range(16)]
        for tc_ in range(4 if dbg != 4 else 0):
            tsl = slice(tc_ * 512, (tc_ + 1) * 512)
            for c in range(16):
                k.op("pe", lambda e: e.matmul(PS[0][:], lhsT=wqh[:, c, :], rhs=uT[:, c, tsl], start=(c == 0), stop=(c == 15)), r=RU + [("wqh", hb)], w=[("ps", 0)])
            k.op("act", lambda e: e.activation(out=qn[:, tsl], in_=PS[0][:], func=AF.Copy, scale=MOBA_SCALE), r=[("ps", 0)], w=[KH + ("qn", tc_), "psrd0"])
            if dbg != 1:
                k.op("dve", lambda e: e.tensor_copy(out=q32[:, tsl], in_=PS[0][:]), r=[("ps", 0)], w=[KH + ("q32", tc_), "psrd0"])
            for c in range(16):
                k.op("pe", lambda e: e.matmul(PS[1][:], lhsT=wkh[:, c, :], rhs=uT[:, c, tsl], start=(c == 0), stop=(c == 15)), r=RU + [("wkh", hb)], w=[("ps", 1)])
            k.op("act", lambda e: e.copy(out=kn[:, tsl], in_=PS[1][:]), r=[("ps", 1)], w=[KH + ("kn", tc_), "psrd1"])
            for bb in range(2 if dbg != 2 else 0):
                k.op("dve", lambda e: e.reduce_sum(out=kmean[:, tc_ * 2 + bb:tc_ * 2 + bb + 1], in_=PS[1][:, bb * 256:(bb + 1) * 256], axis=AX.X),
                     r=[("ps", 1)], w=[("kmean", tc_), "psrd1"])
        KM = [("kmean", t) for t in range(4)]
        if stop == 2:
            t = k.dma("sp", oT[0:128, :], qn, r=[KH + ("qn", t_) for t_ in range(4)] + KM + [KH + ("q32", t_) for t_ in range(4)] + [KH + ("kn", t_) for t_ in range(4)])
            k.finish([t]); k.close(); return nc
        for qt in range(nqt):
            qsl = slice(qt * 128, (qt + 1) * 128)
            nk = (qt + 1) * 128
            nch = (nk + 511) // 512
            qb = qt // 2
            pb = (h * nqt + qt) % 2
            P = arena(o_P + pb * 1024, 2048, BF16)
            KP = ("P", pb)
            rq = [KH + ("qn", qt // 4)]
            for c in range(nch):
                w_ = min(512, nk - c * 512)
                ksl = slice(c * 512, c * 512 + w_)
                k.op("pe", lambda e: e.matmul(PS[c][:, 0:w_], lhsT=qn[:, qsl], rhs=kn[:, ksl], start=True, stop=True), r=rq + [KH + ("kn", c)], w=[("ps", c)])
            if qb >= 1:
                k.op("pe", lambda e: e.matmul(PS[4][:, 0:8], lhsT=q32[:, qsl], rhs=kmean[:], start=True, stop=True), r=[KH + ("q32", qt // 4)] + KM, w=[("ps", 4)])
                k.op("dve", lambda e: e.memset(gate[:], -1e30), w=["gate"])
                k.op("dve", lambda e: e.tensor_copy(out=gate[:, 0:qb], in_=PS[4][:, 0:qb]), r=[("ps", 4), "gate"], w=["gate"])
                k.op("dve", lambda e: e.max(out=m8[:], in_=gate[:]), r=["gate"], w=["m8"])
                k.op("dve", lambda e: e.tensor_scalar(out=selb[:], in0=gate[:], scalar1=m8[:, 2:3], scalar2=None, op0=ALU.is_ge), r=["gate", "m8"], w=["selb"])
                k.op("dve", lambda e: e.tensor_scalar(out=selb[:], in0=selb[:], scalar1=-MOBA_NEG, scalar2=MOBA_NEG, op0=ALU.mult, op1=ALU.add), r=["selb"], w=["selb"])
                k.op("dve", lambda e: e.tensor_scalar(out=selb[:], in0=selb[:], scalar1=cfs[:, h:h + 1], scalar2=None, op0=ALU.add), r=["selb", "cfs"], w=["selb"])
            for n in range(qb + 1):
                c, off = n // 2, (n % 2) * 256
                w_ = min(256, nk - n * 256)
                src = PS[c][:, off:off + w_]
                dst = L[:, n * 256:n * 256 + w_]
                if n < qb:
                    k.op("dve", lambda e: e.tensor_scalar(out=dst, in0=src, scalar1=selb[:, n:n + 1], scalar2=None, op0=ALU.add), r=[("ps", c), "selb"], w=["L"])
                else:
                    k.op("dve", lambda e: e.tensor_scalar(out=dst, in0=src, scalar1=cfs[:, h:h + 1], scalar2=None, op0=ALU.add), r=[("ps", c), "cfs"], w=["L"])
            lc = nch - 1
            wl = nk - lc * 512
            k.op("dve", lambda e: e.tensor_tensor(out=L[:, nk - 128:nk], in0=PS[lc][:, wl - 128:wl], in1=tbs[:, h, 0, :], op=ALU.add), r=[("ps", lc), "tbs", "L"], w=["L"])
            if qt >= 1:
                k.op("dve", lambda e: e.tensor_tensor(out=L[:, nk - 256:nk - 128], in0=L[:, nk - 256:nk - 128], in1=tbs[:, h, 1, :], op=ALU.add), r=["tbs", "L"], w=["L"])
            k.op("dve", lambda e: e.reduce_max(out=mx[:], in_=L[:, 0:nk], axis=AX.X), r=["L"], w=["mx"])
            k.op("dve", lambda e: e.tensor_scalar_mul(out=mx[:], in0=mx[:], scalar1=-1.0), r=["mx"], w=["mx"])
            k.op("act", lambda e: e.activation(out=P[:, 0:nk], in_=L[:, 0:nk], func=AF.Exp, bias=mx[:, 0:1], scale=1.0, accum_out=rsum[:, 0:1]),
                 r=["L", "mx"], w=[KP, "rsum"])
            k.op("dve", lambda e: e.reciprocal(out=rinv[:], in_=rsum[:, 0:1]), r=["rsum"], w=["rinv"])
            k.op("dve", lambda e: e.tensor_scalar_mul(out=dg[:], in0=ident[:], scalar1=rinv[:, 0:1]), r=["ident", "rinv"], w=["dg"])
            nkt = nk // 128
            for g0 in range(0, nkt, 4):
                gi = (g0 // 4) % 2
                PTs = arena(o_PT + gi * 256, 512, BF16).rearrange("p (j q) -> p j q", j=4)
                n_ = min(4, nkt - g0)
                for j in range(n_):
                    kt = g0 + j
                    k.op("pe", lambda e: e.matmul(PS[5 + gi][:, j * 128:(j + 1) * 128], lhsT=P[:, kt * 128:(kt + 1) * 128], rhs=dg[:], start=True, stop=True),
                         r=[KP, "dg"], w=[("ps", 5 + gi)])
                k.op("act", lambda e: e.copy(out=PTs[:, 0:n_, :], in_=PS[5 + gi][:, 0:n_ * 128].rearrange("p (j q) -> p j q", j=n_)), r=[("ps", 5 + gi)], w=[("PT", gi)])
                for j in range(n_):
                    kt = g0 + j
                    k.op("pe", lambda e: e.matmul(PS[7][:, 0:128], lhsT=Vall[:, kt, h * 128:(h + 1) * 128], rhs=PTs[:, j, :], start=(kt == 0), stop=(kt == nkt - 1)),
                         r=[("PT", gi)], w=[("ps", 7)])
            k.op("act", lambda e: e.copy(out=osb[:, qsl], in_=PS[7][:, 0:128]), r=[("ps", 7)], w=[("osb", hb)])
        outs.append(k.dma("sp", oT[h * 128:(h + 1) * 128, 0:nqt * 128], osb[:, 0:nqt * 128], r=[("osb", hb)]))
    k.finish(outs)
    k.close()
    return nc

def build_mod():
    k = KB()
    nc = k.nc
    cT = k.dram("cT", [128, 16, 4])
    W = k.dram("W", [12, 2048, 512])
    ab = k.dram("ab", [128, 12, 4])
    out = k.dram("modT", [128, 48, 4], kind="ExternalOutput")
    cs_ = k.sb("cs_", [128, 16, 4])
    abs_ = k.sb("abs_", [128, 12, 4])
    res = k.sb("res", [128, 48, 4])
    Wb = [k.sb(f"Wb{i}", [128, 16, 512]) for i in range(2)]
    PS = [k.ps(f"ps{i}", [128, 512]) for i in range(2)]
    k.dma("sp", cs_[:], cT[:, :, :], w=["c"])
    k.dma("sp", abs_[:], ab[:, :, :], w=["ab"])
    k.op("act", lambda e: e.activation(out=cs_[:], in_=cs_[:], func=AF.Silu), r=["c"], w=["c"])
    for g in range(12):
        wb = Wb[g % 2]
        k.dma("sp" if g % 2 == 0 else "act", wb[:], W[g].rearrange("(c p) n -> p c n", p=128), w=[("W", g % 2)])
        for j in range(4):
            pi = j % 2
            for c in range(16):
                k.op("pe", lambda e: e.matmul(PS[pi][:, 0:4], lhsT=wb[:, c, j * 128:(j + 1) * 128], rhs=cs_[:, c, :], start=(c == 0), stop=(c == 15)),
                     r=["c", ("W", g % 2)], w=[("ps", pi)])
            k.op("dve", lambda e: e.tensor_scalar(out=res[:, g * 4 + j, :], in0=PS[pi][:, 0:4], scalar1=abs_[:, g, j:j + 1], scalar2=None, op0=ALU.add),
                 r=[("ps", pi), "ab"], w=["res"])
    t = k.dma("sp", out[:, :, :], res[:], r=["res"])
    k.finish([t])
    k.close()
    return nc


def _ppl(v, n=16):
    return np.ascontiguousarray(np.asarray(v).reshape(n, 128).T)


def _c(a):
    return np.ascontiguousarray(a)


def _t5_bucket_np(n):
    n = np.maximum(n, 0)
    large = 16 + (np.log(np.maximum(n, 1).astype(np.float32) / 16) / np.log(128 / 16) * 16).astype(np.int32)
    large = np.minimum(large, 31)
    return np.where(n < 16, n, large)


def _rope_tabs():
    half = 32
    inv = 10000.0 ** (-np.arange(half, dtype=np.float32) / half)
    ang = np.arange(2048, dtype=np.float32)[None, :] * inv[:, None]
    return _c(np.stack([np.cos(ang), np.sin(ang)], 1).astype(np.float32))


def _chunk_consts_gla():
    p = np.arange(128)
    same = (p[:, None] // 64) == (p[None, :] // 64)
    U = (same & (p[:, None] <= p[None, :])).astype(np.float32)
    O = same.astype(np.float32)
    tri = np.zeros((128, 128), np.float32)
    tri[:, :64] = ((p[:, None] % 64) <= np.arange(64)[None, :]).astype(np.float32)
    return _c(np.stack([U, O, tri], 1))


def _chunk_consts_gdn():
    p = np.arange(128)
    same = (p[:, None] // 64) == (p[None, :] // 64)
    U = (same & (p[:, None] <= p[None, :])).astype(np.float32)
    O = same.astype(np.float32)
    oc0 = np.broadcast_to((p < 64)[:, None], (128, 128)).astype(np.float32)
    oc1 = np.broadcast_to((p >= 64)[:, None], (128, 128)).astype(np.float32)
    ident = np.eye(128, dtype=np.float32)
    i64 = np.arange(64)[None, :]
    jl = (p % 64)[:, None]
    c5 = np.concatenate([(jl == i64), (jl <= i64)], 1).astype(np.float32)
    c6 = np.concatenate([(jl < i64), np.zeros((128, 64), bool)], 1).astype(np.float32)
    return _c(np.stack([U, O, oc0, oc1, ident, c5, c6], 1))


def _prep_mla(I, hh, xT_b, pp):
    qi = np.arange(128)[:, None]; ki = np.arange(128)[None, :]
    cmask = np.where(ki <= qi, 0.0, -30000.0).astype(np.float32)
    nrm = _c(np.stack([_ppl(I["mla_q_norm"][0], 4), _ppl(I["mla_kv_norm"][0], 4)], 1).astype(np.float32))
    return {"xT": xT_b, "pp": pp, "w_in": I["mla_w_in"][0], "nrm": nrm,
            "w_qb": _c(I["mla_w_qb"][0][:, hh * 1536:(hh + 1) * 1536]),
            "w_kvb": _c(I["mla_w_kvb"][0][:, hh * 2048:(hh + 1) * 2048]),
            "cs": _rope_tabs(), "cmask": cmask}


def _prep_gdn(I, hh, xT_b, pp):
    w = I["gdn_w_in"][0]
    cwf = I["gdn_conv_w"][0]
    qc = slice(hh * 1024, (hh + 1) * 1024); kc = slice(2048 + hh * 1024, 2048 + (hh + 1) * 1024); vc = slice(4096 + hh * 2048, 4096 + (hh + 1) * 2048)
    cwm = np.concatenate([cwf[:, qc], cwf[:, kc], cwf[:, vc]], 1)
    cw = cwm.T.reshape(32, 128, 4).transpose(1, 0, 2)
    hs = slice(hh * 16, (hh + 1) * 16)
    hv = np.stack([np.broadcast_to(I["gdn_a_log"][0][hs][None, :], (128, 16)), np.broadcast_to(I["gdn_dt_bias"][0][hs][None, :], (128, 16))], 1)
    wba = np.concatenate([w[:, 12288 + hh * 16:12288 + (hh + 1) * 16], w[:, 12320 + hh * 16:12320 + (hh + 1) * 16]], 1)
    return {"xT": xT_b, "pp": pp, "wq": _c(w[:, qc]), "wk": _c(w[:, kc]), "wv": _c(w[:, vc]),
            "wz": _c(w[:, 8192 + hh * 2048:8192 + (hh + 1) * 2048]), "wba": _c(wba), "cw": _c(cw.astype(np.float32)),
            "hv": _c(hv.astype(np.float32)), "ngp": _c(I["gdn_norm"][0][:, None].astype(np.float32)), "cst": _chunk_consts_gdn()}


def _prep_gla(I, hh, xT_b, pp):
    w = I["gla_w_in"][0]
    wgk = np.concatenate([I["gla_w_gk"][0][:, hh * 512:(hh + 1) * 512], I["gla_b_gk"][0][None, hh * 512:(hh + 1) * 512]], 0)
    return {"xT": xT_b, "pp": pp,
            "w_q": _c(w[:, hh * 512:(hh + 1) * 512]), "w_k": _c(w[:, 1024 + hh * 512:1024 + (hh + 1) * 512]),
            "w_v": _c(w[:, 2048 + hh * 1024:2048 + (hh + 1) * 1024]), "w_g": _c(w[:, 4096 + hh * 1024:4096 + (hh + 1) * 1024]),
            "w_gl": _c(w[:, 6144:6160]), "w_gk": _c(wgk.astype(np.float32)),
            "ngb": _c(np.broadcast_to(I["gla_norm"][0][None, :], (128, 512))), "cst": _chunk_consts_gla()}


def _prep_moba(I, hh, xT_b, pp):
    w = I["moba_w_in"][0]
    hs = slice(hh * 1024, (hh + 1) * 1024)
    qi = np.arange(128)[:, None]; ki = np.arange(128)[None, :]
    rb = I["rel_bias"]
    heads = np.arange(hh * 8, hh * 8 + 8)
    b0 = _t5_bucket_np(qi - ki); b1 = _t5_bucket_np(128 + qi - ki)
    T0 = rb[b0][:, :, heads]
    T0 = np.where((ki <= qi)[:, :, None], T0, np.float32(-30000.0))
    T1 = rb[b1][:, :, heads]
    tb = np.stack([T0.transpose(0, 2, 1), T1.transpose(0, 2, 1)], 2).astype(np.float32)
    cf = np.broadcast_to(rb[31, heads][None, :], (128, 8)).astype(np.float32)
    return {"xT": xT_b, "pp": pp, "w_q": _c(w[:, 0:2048][:, hs]), "w_k": _c(w[:, 2048:4096][:, hs]),
            "w_v": _c(w[:, 4096:6144][:, hs]), "tb": _c(tb), "cf": _c(cf)}


_NC_CACHE = {}


def _get_nc(name, fn):
    if name not in _NC_CACHE:
        _NC_CACHE[name] = fn()
    return _NC_CACHE[name]


def kernel(**inputs):
    I = {k_: np.asarray(v) for k_, v in inputs.items()}
    cores = list(range(8))
    cT = _c(I["c"].T.reshape(16, 128, 4).transpose(1, 0, 2).astype(np.float32))
    in_maps = []
    for c in cores:
        Ws, abs_ = [], []
        for g in range(12 * c, 12 * c + 12):
            layer, col0 = g // 24, (g % 24) * 512
            Ws.append(I["ada_w"][layer][:, col0:col0 + 512])
            abs_.append(I["ada_b"][layer][col0:col0 + 512].reshape(4, 128).T)
        in_maps.append({"cT": cT, "W": _c(np.stack(Ws, 0)), "ab": _c(np.stack(abs_, 1).astype(np.float32))})
    res = run_bass_kernel_spmd(_get_nc("mod", build_mod), in_maps, core_ids=cores)
    modT = np.zeros((4, 128, 96, 4), np.float32)
    for c in cores:
        r = res.results[c]["modT"]
        for gi in range(12):
            g = 12 * c + gi
            layer, blk0 = g // 24, (g % 24) * 4
            modT[layer][:, blk0:blk0 + 4, :] = r[:, gi * 4:(gi + 1) * 4, :]

    def vec(layer, b, which):
        return modT[layer][:, which * 16:(which + 1) * 16, b]

    xT = [_c(I["x"][b].T) for b in range(4)]
    preps = [_prep_mla, _prep_gdn, _prep_gla, _prep_moba]
    builders = [("mla", build_A_mla), ("gdn", build_A_gdn), ("gla", build_A_gla), ("moba", build_A_moba)]
    w_os = [I["mla_w_o"][0], I["gdn_w_o"][0], I["gla_w_o"][0], I["moba_w_o"][0]]
    for i in range(4):
        in_maps = []
        for c in cores:
            b, hh = c // 2, c % 2
            pp = _c(np.stack([vec(i, b, 1), vec(i, b, 0)], 1).astype(np.float32))
            in_maps.append(preps[i](I, hh, xT[b], pp))
        res = run_bass_kernel_spmd(_get_nc(*builders[i]), in_maps, core_ids=cores)
        oTb = []
        for b in range(4):
            if i == 2:
                halves = [np.asarray(res.results[2 * b + hh]["o_tok"]).T for hh in range(2)]
            else:
                halves = [np.asarray(res.results[2 * b + hh]["oT"]) for hh in range(2)]
            oTb.append(np.concatenate(halves, 0))
        Kdim = oTb[0].shape[0]
        in_maps = []
        rb = _c(np.broadcast_to(I["router_b"][i][None, :], (128, 32)).astype(np.float32))
        bgu = _c(I["moe_b_gu"][i].reshape(32, 12, 128).transpose(2, 0, 1))
        for c in cores:
            b, hf = c // 2, c % 2
            sl = slice(hf * 1024, (hf + 1) * 1024)
            pp = np.stack([vec(i, b, 2), vec(i, b, 4), vec(i, b, 3), vec(i, b, 5), _ppl(I["ln_g"][i, 0]), _ppl(I["ln_b"][i, 0]),
                           _ppl(I["ln_g"][i, 1]), _ppl(I["ln_b"][i, 1])], 1).astype(np.float32)
            in_maps.append({"oT": _c(oTb[b][:, sl]), "xT": _c(xT[b][:, sl]), "w_o": w_os[i], "pp": _c(pp), "rw": I["router_w"][i], "rb": rb,
                            "wgu": I["moe_w_gu"][i], "bgu": bgu, "wdn": I["moe_w_down"][i], "bdn": I["moe_b_down"][i]})
        res = run_bass_kernel_spmd(_get_nc("B%d" % Kdim, lambda: build_B(Kdim)), in_maps, core_ids=cores)
        xT = [_c(np.concatenate([np.asarray(res.results[2 * b + hf]["outT"]) for hf in range(2)], 1)) for b in range(4)]
    return _c(np.stack([xT[b].T for b in range(4)], 0).astype(np.float32))
```
